# Optimizing a Trainium2 kernel written in Bass

```python
import jax
import jax.numpy as jnp
from jax import lax
import numpy as np


D_MODEL = 4096
BATCH = 2
SEQ = 8192
DEPTH = 4

GRID_W = 64
CTX_LEN = 256
N_MIXERS = 4
MIX_WIDTH = D_MODEL
GROUP_WIDTH = MIX_WIDTH // N_MIXERS
N_MOD = 6
NORM_EPS = 1e-6

HG_HEAD_DIM = 128
HG_HEADS = GROUP_WIDTH // HG_HEAD_DIM
HG_CHUNK = 64
HG_IN = 5 * GROUP_WIDTH

RW_HEAD_DIM = 64
RW_HEADS = GROUP_WIDTH // RW_HEAD_DIM
RW_DECAY_RANK = 64
RW_AAA_RANK = 64
RW_GATE_RANK = 160
RW_GN_EPS = 64e-5
RW_SPLITS = (GROUP_WIDTH, GROUP_WIDTH, GROUP_WIDTH, RW_DECAY_RANK, RW_DECAY_RANK, RW_AAA_RANK, RW_AAA_RANK, RW_GATE_RANK)
RW_IN = 3 * GROUP_WIDTH + 2 * RW_DECAY_RANK + 2 * RW_AAA_RANK + RW_GATE_RANK

LRU_HEADS = 8
LRU_BLOCK = GROUP_WIDTH // LRU_HEADS
LRU_CONV = 4
LRU_C = 8.0
LRU_IN = 2 * GROUP_WIDTH

POOL_WINDOWS = (2, 4, 8, 16)
POOL_GROUP = GROUP_WIDTH // 4
POOL_IN = GROUP_WIDTH

MIXER_IN_WIDTHS = (HG_IN, RW_IN, LRU_IN, POOL_IN)
IN_WIDTH = HG_IN + RW_IN + LRU_IN + POOL_IN

N_EXPERT_GROUPS = 4
EXPERTS_PER_GROUP = 4
N_EXPERTS = N_EXPERT_GROUPS * EXPERTS_PER_GROUP
TOP_K = 2
EXPERT_HIDDEN = 256

kernel_name = 'hybrid_hgrn2_rwkv7_rglru_pool_hmoe_dit'


def split_cols(z, widths):
    return jnp.split(z, np.cumsum(widths)[:-1].tolist(), axis=-1)


def rmsnorm(x, g):
    xf = x.astype(jnp.float32)
    y = xf * lax.rsqrt(jnp.mean(xf * xf, axis=-1, keepdims=True) + NORM_EPS)
    return (y * g.astype(jnp.float32)).astype(x.dtype)


def modulate(h, shift, scale):
    return h * (1.0 + scale) + shift


def flip(t):
    return jnp.flip(t, axis=1)


def gla_chunk_scan(q, k, v, log_f, s0):
    b_, l_, h_, _ = q.shape
    n_chunks = l_ // HG_CHUNK

    def to_chunks(t):
        t = t.reshape(b_, n_chunks, HG_CHUNK, h_, t.shape[-1])
        return jnp.transpose(t, (1, 0, 3, 2, 4))

    lower = jnp.tril(jnp.ones((HG_CHUNK, HG_CHUNK), dtype=bool))[:, :, None]

    def step(s, inp):
        qc, kc, vc, gc = inp
        cum = jnp.cumsum(gc, axis=2)
        diff = cum[:, :, :, None, :] - cum[:, :, None, :, :]
        decay = jnp.where(lower, jnp.exp(jnp.where(lower, diff, 0.0)), 0.0)
        scores = jnp.einsum('bhtk,bhsk,bhtsk->bhts', qc, kc, decay)
        o = jnp.einsum('bhts,bhsv->bhtv', scores, vc) + jnp.einsum('bhtk,bhkv->bhtv', qc * jnp.exp(cum), s)
        total = cum[:, :, -1:, :]
        s_new = jnp.exp(total[:, :, 0, :])[..., None] * s + jnp.einsum('bhsk,bhsv->bhkv', kc * jnp.exp(total - cum), vc)
        return s_new, o

    s_last, o = lax.scan(step, s0, (to_chunks(q), to_chunks(k), to_chunks(v), to_chunks(log_f)))
    o = jnp.transpose(o, (1, 0, 3, 2, 4)).reshape(b_, l_, h_, v.shape[-1])
    return o, s_last


def rwkv7_scan(r, log_w, k, v, kk, a, s0):
    def step(s, inp):
        r_t, lw_t, k_t, v_t, kk_t, a_t = inp
        sa = jnp.einsum('bhvk,bhk->bhv', s, -kk_t)
        s = s * jnp.exp(lw_t)[:, :, None, :] + sa[..., None] * (kk_t * a_t)[:, :, None, :] + v_t[..., None] * k_t[:, :, None, :]
        return s, jnp.einsum('bhvk,bhk->bhv', s, r_t)

    xs = tuple(jnp.moveaxis(t, 1, 0) for t in (r, log_w, k, v, kk, a))
    s_last, y = lax.scan(step, s0, xs)
    return jnp.moveaxis(y, 0, 1), s_last


def linear_scan(a, b, h0):
    def combine(e1, e2):
        return e1[0] * e2[0], e2[0] * e1[1] + e2[1]

    a_cum, b_cum = lax.associative_scan(combine, (a, b), axis=1)
    return a_cum * h0[:, None, :] + b_cum


def token_shift(z, mu):
    zp = jnp.pad(z, ((0, 0), (1, 1), (0, 0)))
    return z + mu * (0.5 * (zp[:, :-2] + zp[:, 2:]) - z)


def conv_centred(x, w, b):
    left = LRU_CONV // 2
    n = x.shape[1]
    xp = jnp.pad(x, ((0, 0), (left, LRU_CONV - 1 - left), (0, 0)))
    return b + sum(w[j] * xp[:, j:j + n] for j in range(LRU_CONV))


def blockdiag(x, w):
    xh = x.reshape(x.shape[:-1] + (LRU_HEADS, LRU_BLOCK))
    return jnp.einsum('blhi,hij->blhj', xh, w).reshape(x.shape)


def centred_mean(x, w, axis):
    xm = jnp.moveaxis(x, axis, -2).astype(jnp.float32)
    n = xm.shape[-2]
    cs = jnp.cumsum(xm, axis=-2)
    cs = jnp.concatenate([jnp.zeros_like(cs[..., :1, :]), cs], axis=-2)
    t = jnp.arange(n)
    lo = jnp.clip(t - w // 2, 0, n)
    hi = jnp.clip(t + w // 2, 0, n)
    s = jnp.take(cs, hi, axis=-2) - jnp.take(cs, lo, axis=-2)
    m = s / (hi - lo).astype(jnp.float32)[:, None]
    return jnp.moveaxis(m.astype(x.dtype), -2, axis)


def hgrn2_mixer(zc, zl, lb_f, lb_b, gnorm_w, need_ctx):
    def heads(t):
        return t.reshape(t.shape[:-1] + (HG_HEADS, HG_HEAD_DIM))

    def prep(z):
        q, f_f, f_b, i, g = jnp.split(z.astype(jnp.float32), 5, axis=-1)
        q = jax.nn.silu(q) * HG_HEAD_DIM ** -0.5
        f_f = lb_f + (1.0 - lb_f) * jax.nn.sigmoid(f_f)
        f_b = lb_b + (1.0 - lb_b) * jax.nn.sigmoid(f_b)
        return (heads(q), heads(1.0 - f_f), heads(jnp.log(f_f)), heads(1.0 - f_b), heads(jnp.log(f_b)), heads(i), g)

    def bidir(p, s_f, s_b):
        q, k_f, lf_f, k_b, lf_b, v, _ = p
        o_f, s_f = gla_chunk_scan(q, k_f, v, lf_f, s_f)
        o_b, s_b = gla_chunk_scan(flip(q), flip(k_b), flip(v), flip(lf_b), s_b)
        return o_f + flip(o_b), s_f, s_b

    def readout(o, p):
        g = p[-1]
        o = o * lax.rsqrt(jnp.mean(o * o, axis=-1, keepdims=True) + NORM_EPS) * gnorm_w
        return o.reshape(g.shape) * jax.nn.silu(g)

    pc, pl = prep(zc), prep(zl)
    s0 = jnp.zeros((zc.shape[0], HG_HEADS, HG_HEAD_DIM, HG_HEAD_DIM), jnp.float32)
    oc, s_f, s_b = bidir(pc, s0, s0)
    ol, _, _ = bidir(pl, s_f, s_b)
    return (readout(oc, pc) if need_ctx else None), readout(ol, pl)


def rwkv7_mixer(zc, zl, mu, w0, w_up, a0, a_up, g_up, k_k, k_a, r_k, ln_w, ln_b, need_ctx):
    def heads(t):
        return t.reshape(t.shape[:-1] + (RW_HEADS, RW_HEAD_DIM))

    def prep(z):
        z = token_shift(z.astype(jnp.float32), mu)
        r, k, v, wd_f, wd_b, ad_f, ad_b, gd = split_cols(z, RW_SPLITS)
        log_w = [heads(-jnp.exp(-jax.nn.softplus(-(w0[d] + jnp.tanh(wd) @ w_up[d])) - 0.5))
                 for d, wd in enumerate((wd_f, wd_b))]
        a = [jax.nn.sigmoid(a0[d] + ad @ a_up[d]) for d, ad in enumerate((ad_f, ad_b))]
        kk = heads(k * k_k)
        kk = kk * lax.rsqrt(jnp.sum(kk * kk, axis=-1, keepdims=True) + 1e-12)
        ks = [heads(k * (1.0 + (ad - 1.0) * k_a)) for ad in a]
        return heads(r), log_w, ks, heads(v), kk, [heads(ad) for ad in a], jax.nn.sigmoid(gd) @ g_up

    def bidir(p, s_f, s_b):
        r, log_w, ks, v, kk, a, _ = p
        y_f, s_f = rwkv7_scan(r, log_w[0], ks[0], v, kk, a[0], s_f)
        y_b, s_b = rwkv7_scan(flip(r), flip(log_w[1]), flip(ks[1]), flip(v), flip(kk), flip(a[1]), s_b)
        return y_f + flip(y_b), s_f, s_b

    def readout(y, p):
        r, _, ks, v, _, _, g = p
        yc = y - jnp.mean(y, axis=-1, keepdims=True)
        yn = yc * lax.rsqrt(jnp.mean(yc * yc, axis=-1, keepdims=True) + RW_GN_EPS)
        bonus = jnp.sum(r * (ks[0] + ks[1]) * heads(r_k), axis=-1, keepdims=True) * v
        return (yn.reshape(g.shape) * ln_w + ln_b + bonus.reshape(g.shape)) * g

    pc, pl = prep(zc), prep(zl)
    s0 = jnp.zeros((zc.shape[0], RW_HEADS, RW_HEAD_DIM, RW_HEAD_DIM), jnp.float32)
    yc_, s_f, s_b = bidir(pc, s0, s0)
    yl_, _, _ = bidir(pl, s_f, s_b)
    return (readout(yc_, pc) if need_ctx else None), readout(yl_, pl)


def rglru_mixer(zc, zl, conv_w, conv_b, wa, ba, wx, bx, lam, need_ctx):
    def prep(z):
        xb, yg = jnp.split(z.astype(jnp.float32), 2, axis=-1)
        xb = conv_centred(xb, conv_w, conv_b)
        dirs = []
        for d in range(2):
            r = jax.nn.sigmoid(blockdiag(xb, wa[d]) + ba[d])
            i = jax.nn.sigmoid(blockdiag(xb, wx[d]) + bx[d])
            log_a = -LRU_C * r * jax.nn.softplus(-lam[d])
            dirs.append((jnp.exp(log_a), jnp.sqrt(-jnp.expm1(2.0 * log_a)) * i * xb))
        return dirs, yg

    def bidir(p, h_f, h_b):
        (a_f, b_f), (a_b, b_b) = p[0]
        o_f = linear_scan(a_f, b_f, h_f)
        o_b = flip(linear_scan(flip(a_b), flip(b_b), h_b))
        return o_f + o_b, o_f[:, -1], o_b[:, 0]

    def readout(o, p):
        return o * jax.nn.gelu(p[1], approximate=True)

    pc, pl = prep(zc), prep(zl)
    h0 = jnp.zeros((zc.shape[0], GROUP_WIDTH), jnp.float32)
    oc, h_f, h_b = bidir(pc, h0, h0)
    ol, _, _ = bidir(pl, h_f, h_b)
    return (readout(oc, pc) if need_ctx else None), readout(ol, pl)


def pool_mixer(p, lin_w, scale, axis):
    groups = jnp.split(p, len(POOL_WINDOWS), axis=-1)
    outs = [jnp.einsum('...i,ij->...j', centred_mean(gx, win, axis) - gx, lin_w[gi])
            for gi, (gx, win) in enumerate(zip(groups, POOL_WINDOWS))]
    return jnp.concatenate(outs, axis=-1) * scale


def token_mixers(zc, zl, rows, pool_axis, hg_p, rw_p, lru_p, pool_p, need_ctx):
    hg_c, rw_c, lru_c, pool_c = split_cols(zc, MIXER_IN_WIDTHS)
    hg_l, rw_l, lru_l, pool_l = split_cols(zl, MIXER_IN_WIDTHS)
    ya_c, ya_l = hgrn2_mixer(hg_c, hg_l, *hg_p, need_ctx)
    yb_c, yb_l = rwkv7_mixer(rw_c, rw_l, *rw_p, need_ctx)
    yc_c, yc_l = rglru_mixer(lru_c, lru_l, *lru_p, need_ctx)
    b_, l_, w_ = pool_l.shape
    yd_l = pool_mixer(pool_l.reshape(b_, rows, GRID_W, w_), *pool_p, pool_axis).reshape(b_, l_, GROUP_WIDTH)
    y_lat = jnp.concatenate([t.astype(zl.dtype) for t in (ya_l, yb_l, yc_l, yd_l)], axis=-1)
    if not need_ctx:
        return None, y_lat
    yd_c = pool_mixer(pool_c, *pool_p, 1)
    y_ctx = jnp.concatenate([t.astype(zc.dtype) for t in (ya_c, yb_c, yc_c, yd_c)], axis=-1)
    return y_ctx, y_lat


def moe_ffn(h, wc, bc, wf, bf, w_gu, w_down):
    lg = (jnp.einsum('bld,dg->blg', h, wc) + bc).astype(jnp.float32)
    pg = jax.nn.softmax(lg, axis=-1)
    gsel = jnp.argmax(lg, axis=-1)
    gprob = jnp.max(pg, axis=-1, keepdims=True)
    le = (jnp.einsum('bld,de->ble', h, wf) + bf).astype(jnp.float32)
    le = le.reshape(le.shape[:-1] + (N_EXPERT_GROUPS, EXPERTS_PER_GROUP))
    le_sel = jnp.sum(le * jax.nn.one_hot(gsel, N_EXPERT_GROUPS, dtype=jnp.float32)[..., None], axis=-2)
    top_v, top_i = lax.top_k(jax.nn.softmax(le_sel, axis=-1), TOP_K)
    top_v = top_v / jnp.sum(top_v, axis=-1, keepdims=True)
    eid = gsel[..., None] * EXPERTS_PER_GROUP + top_i
    gates = jnp.sum(jax.nn.one_hot(eid, N_EXPERTS, dtype=jnp.float32) * (gprob * top_v)[..., None], axis=-2)
    gt, up = jnp.split(jnp.einsum('bld,edh->bleh', h, w_gu), 2, axis=-1)
    act = jax.nn.silu(gt) * up * gates[..., None].astype(h.dtype)
    return jnp.einsum('bleh,ehd->bld', act, w_down)


def setup_inputs(seed: int = 0) -> dict:
    key = jax.random.key(seed)
    keys = iter(jax.random.split(key, 64))
    f32 = jnp.float32
    gw = GROUP_WIDTH

    def nrm(shape, scale):
        return scale * jax.random.normal(next(keys), shape, f32)

    def unif(shape, lo, hi):
        return jax.random.uniform(next(keys), shape, f32, lo, hi)

    a_base = unif((DEPTH, 2, gw), 0.9, 0.999) ** (1.0 / LRU_C)
    return {
        'x': nrm((BATCH, SEQ, D_MODEL), 1.0),
        'c': nrm((BATCH, D_MODEL), 1.0),
        'ctx': nrm((BATCH, CTX_LEN, D_MODEL), 1.0),
        'c_ctx': nrm((D_MODEL,), 1.0),
        'w_ada': nrm((DEPTH, D_MODEL, N_MOD * D_MODEL), 0.5 * D_MODEL ** -0.5),
        'b_ada': nrm((DEPTH, N_MOD * D_MODEL), 0.02),
        'norm_g': 1.0 + nrm((DEPTH, 2, D_MODEL), 0.02),
        'w_in': nrm((DEPTH, D_MODEL, IN_WIDTH), D_MODEL ** -0.5),
        'w_out': nrm((DEPTH, MIX_WIDTH, D_MODEL), MIX_WIDTH ** -0.5),
        'hg_lb': nrm((2, DEPTH, gw), 0.1),
        'hg_gnorm': 1.0 + nrm((DEPTH, HG_HEAD_DIM), 0.02),
        'rw_mu': unif((DEPTH, RW_IN), 0.0, 1.0),
        'rw_w0': unif((DEPTH, 2, gw), -6.0, -1.0),
        'rw_w_up': nrm((DEPTH, 2, RW_DECAY_RANK, gw), 0.1),
        'rw_a0': nrm((DEPTH, 2, gw), 0.5),
        'rw_a_up': nrm((DEPTH, 2, RW_AAA_RANK, gw), 0.1),
        'rw_g_up': nrm((DEPTH, RW_GATE_RANK, gw), RW_GATE_RANK ** -0.5),
        'rw_kk': 0.85 + nrm((DEPTH, gw), 0.05),
        'rw_ka': 1.0 + nrm((DEPTH, gw), 0.05),
        'rw_rk': nrm((DEPTH, gw), 0.1),
        'rw_ln_w': 1.0 + nrm((DEPTH, gw), 0.02),
        'rw_ln_b': nrm((DEPTH, gw), 0.02),
        'lru_conv_w': nrm((DEPTH, LRU_CONV, gw), LRU_CONV ** -0.5),
        'lru_conv_b': nrm((DEPTH, gw), 0.02),
        'lru_wa': nrm((DEPTH, 2, LRU_HEADS, LRU_BLOCK, LRU_BLOCK), LRU_BLOCK ** -0.5),
        'lru_ba': nrm((DEPTH, 2, gw), 0.02),
        'lru_wx': nrm((DEPTH, 2, LRU_HEADS, LRU_BLOCK, LRU_BLOCK), LRU_BLOCK ** -0.5),
        'lru_bx': nrm((DEPTH, 2, gw), 0.02),
        'lru_lam': jnp.log(a_base) - jnp.log1p(-a_base),
        'pool_w': nrm((DEPTH, len(POOL_WINDOWS), POOL_GROUP, POOL_GROUP), POOL_GROUP ** -0.5),
        'pool_scale': 1.0 + nrm((DEPTH, gw), 0.1),
        'moe_wc': nrm((DEPTH, D_MODEL, N_EXPERT_GROUPS), D_MODEL ** -0.5),
        'moe_bc': nrm((DEPTH, N_EXPERT_GROUPS), 0.01),
        'moe_wf': nrm((DEPTH, D_MODEL, N_EXPERTS), D_MODEL ** -0.5),
        'moe_bf': nrm((DEPTH, N_EXPERTS), 0.01),
        'moe_w_gu': nrm((DEPTH, N_EXPERTS, D_MODEL, 2 * EXPERT_HIDDEN), D_MODEL ** -0.5),
        'moe_w_down': nrm((DEPTH, N_EXPERTS, EXPERT_HIDDEN, D_MODEL), EXPERT_HIDDEN ** -0.5),
        'final_g': 1.0 + nrm((D_MODEL,), 0.02),
    }


def reference(x, c, ctx, c_ctx, w_ada, b_ada, norm_g, w_in, w_out, hg_lb, hg_gnorm, rw_mu, rw_w0, rw_w_up, rw_a0, rw_a_up, rw_g_up, rw_kk, rw_ka, rw_rk, rw_ln_w, rw_ln_b, lru_conv_w, lru_conv_b, lru_wa, lru_ba, lru_wx, lru_bx, lru_lam, pool_w, pool_scale, moe_wc, moe_bc, moe_wf, moe_bf, moe_w_gu, moe_w_down, final_g):
    rows = x.shape[1] // GRID_W
    lb_soft = jax.nn.softmax(hg_lb.astype(jnp.float32), axis=1)
    lower_bounds = jnp.cumsum(lb_soft, axis=1) - lb_soft[:, :1]
    silu_c = jax.nn.silu(c)
    silu_cc = jax.nn.silu(c_ctx)
    xl, xc = x, ctx
    for l in range(DEPTH):
        need_ctx = l < DEPTH - 1
        mod_l = jnp.split((silu_c @ w_ada[l] + b_ada[l])[:, None, :], N_MOD, axis=-1)
        mod_c = jnp.split(silu_cc @ w_ada[l] + b_ada[l], N_MOD, axis=-1)
        zl = modulate(rmsnorm(xl, norm_g[l, 0]), mod_l[0], mod_l[1]) @ w_in[l]
        zc = modulate(rmsnorm(xc, norm_g[l, 0]), mod_c[0], mod_c[1]) @ w_in[l]
        hg_p = (lower_bounds[0, l], lower_bounds[1, l], hg_gnorm[l])
        rw_p = (rw_mu[l], rw_w0[l], rw_w_up[l], rw_a0[l], rw_a_up[l], rw_g_up[l], rw_kk[l], rw_ka[l], rw_rk[l], rw_ln_w[l], rw_ln_b[l])
        lru_p = (lru_conv_w[l], lru_conv_b[l], lru_wa[l], lru_ba[l], lru_wx[l], lru_bx[l], lru_lam[l])
        pool_axis = 2 if l % 2 == 0 else 1
        yc, yl = token_mixers(zc, zl, rows, pool_axis, hg_p, rw_p, lru_p, (pool_w[l], pool_scale[l]), need_ctx)
        ffn_p = (moe_wc[l], moe_bc[l], moe_wf[l], moe_bf[l], moe_w_gu[l], moe_w_down[l])
        xl = xl + mod_l[2] * (yl @ w_out[l])
        xl = xl + mod_l[5] * moe_ffn(modulate(rmsnorm(xl, norm_g[l, 1]), mod_l[3], mod_l[4]), *ffn_p)
        if need_ctx:
            xc = xc + mod_c[2] * (yc @ w_out[l])
            xc = xc + mod_c[5] * moe_ffn(modulate(rmsnorm(xc, norm_g[l, 1]), mod_c[3], mod_c[4]), *ffn_p)
    return rmsnorm(xl, final_g)
```

```python
import numpy as np
import concourse.bass as bass
import concourse.mybir as mybir
from concourse.bass_utils import run_bass_kernel_spmd

F32 = mybir.dt.float32
BF16 = mybir.dt.bfloat16
AF = mybir.ActivationFunctionType
ALU = mybir.AluOpType
AX = mybir.AxisListType


class Tile:
    __slots__ = ("ap", "last_w", "reads", "name", "bank", "pe_last")

    def __init__(self, ap, name="", bank=None):
        self.bank = bank
        self.pe_last = None
        self.ap = ap
        self.last_w = None
        self.reads = {}
        self.name = name

    def __getitem__(self, idx):
        return self.ap[idx]


class KB:
    def __init__(self, same_engine_sync=True, n_dma_sems=12):
        self.nc = bass.Bass("TRN2", target_bir_lowering=False)
        nc = self.nc
        self.eng = {"pe": nc.tensor, "act": nc.scalar, "dve": nc.vector, "pool": nc.gpsimd, "sp": nc.sync}
        self.sem = {}
        self.cnt = {}
        for e in self.eng:
            self.sem[e] = nc.alloc_semaphore("cnt_" + e)
            self.cnt[e] = 0
        self.same = same_engine_sync
        self.dq = {}
        for q in ("sp", "act", "pool"):
            self.dq[q] = {"sems": [nc.alloc_semaphore(f"dma_{q}_{i}") for i in range(n_dma_sems)],
                          "vals": [0] * n_dma_sems, "next": 0}
        self.seen = {e: {} for e in self.eng}
        self.semobj = {}
        for e in self.eng:
            self.semobj[("e", e)] = self.sem[e]
        self.out_events = []
        self.n_instr = 0
        self._uid = 0

    def sbuf(self, shape, dtype=F32, name=None):
        self._uid += 1
        nm = f"sb{self._uid}_{name or ''}"
        t = self.nc.alloc_sbuf_tensor(nm, list(shape), dtype)
        return Tile(t.ap(), nm)

    def psum(self, shape, dtype=F32, name=None):
        self._uid += 1
        nm = f"ps{self._uid}_{name or ''}"
        t = self.nc.alloc_psum_tensor(nm, list(shape), dtype)
        return Tile(t.ap(), nm)

    def dram(self, name, shape, dtype=F32, kind="Internal"):
        t = self.nc.dram_tensor(name, list(shape), dtype, kind=kind)
        return Tile(t.ap(), name)

    def view(self, ap, name=""):
        return Tile(ap, name)

    def _wait(self, e, ev):
        if ev is None:
            return
        key, val = ev
        if key == ("e", e) and not self.same:
            return
        if self.seen[e].get(key, 0) >= val:
            return
        self.eng[e].wait_ge(self.semobj[key], val)
        self.seen[e][key] = val

    def _deps(self, e, reads, writes):
        reads = [t.bank if t.bank is not None else t for t in reads]
        writes = [t.bank if t.bank is not None else t for t in writes]
        for t in reads:
            self._wait(e, t.last_w)
        for t in writes:
            self._wait(e, t.last_w)
            for ev in t.reads.values():
                self._wait(e, ev)

    def _mark(self, ev, reads, writes):
        reads = [t.bank if t.bank is not None else t for t in reads]
        writes = [t.bank if t.bank is not None else t for t in writes]
        for t in reads:
            t.reads[ev[0]] = ev
        for t in writes:
            t.last_w = ev
            t.reads = {}

    def op(self, e, fn, reads=(), writes=()):
        self._deps(e, reads, writes)
        if e == "pe":
            for t in writes:
                if t.bank is not None:
                    self._wait(e, t.bank.pe_last)
        ins = fn(self.eng[e])
        self.cnt[e] += 1
        ins.then_inc(self.sem[e], 1)
        ev = (("e", e), self.cnt[e])
        if e == "pe":
            for t in writes:
                if t.bank is not None:
                    t.bank.pe_last = ev
        self.seen[e][("e", e)] = self.seen[e].get(("e", e), 0)
        self._mark(ev, reads, writes)
        self.n_instr += 1
        return ev

    def dma(self, q, out, in_, reads=(), writes=(), is_output=False):
        self._deps(q, reads, writes)
        d = self.dq[q]
        i = d["next"]
        d["next"] = (i + 1) % len(d["sems"])
        key = ("d", q, i)
        self.semobj[key] = d["sems"][i]
        if d["vals"][i] > 0:
            self._wait(q, (key, d["vals"][i]))
        ins = self.eng[q].dma_start(out=out, in_=in_)
        d["vals"][i] += 16
        ins.then_inc(d["sems"][i], 16)
        ev = (key, d["vals"][i])
        self._mark(ev, reads, writes)
        if is_output:
            self.out_events.append(ev)
        self.n_instr += 1
        return ev

    def finish(self):
        for q, d in self.dq.items():
            for i, v in enumerate(d["vals"]):
                if v > 0:
                    key = ("d", q, i)
                    self._wait("sp", (key, v))
        for e in self.eng:
            if e != "sp" and self.cnt[e] > 0:
                self._wait("sp", (("e", e), self.cnt[e]))


def I(k, e, meth, reads, writes, *a, **kw):
    return k.op(e, lambda eng: getattr(eng, meth)(*a, **kw), reads=reads, writes=writes)


class Cfg:
    def __init__(self, D=4096, IN_W=11680, ntiles_lat=4, nt_a=512, nt_c=256, nctx=64, E=16, EH=256):
        self.D = D
        self.NKC = D // 128
        self.IN_W = IN_W
        self.NCC_IN = -(-IN_W // 128)
        self.nctx = nctx
        self.nlat = ntiles_lat * nt_a
        self.NTOK = self.nlat + nctx
        self.nt_a = nt_a
        self.nt_c = nt_c
        self.E = E
        self.EH = EH
        self.NHC = 2 * EH // 128
        self.NKD = E * EH // 128

    def tiles(self, nt):
        t = [(i * nt, nt, 0) for i in range(self.nlat // nt)]
        t.append((self.nlat, self.nctx, 1))
        return t


def wlayout(W):
    K, C = W.shape
    nkc = K // 128
    ncc = -(-C // 128)
    if ncc * 128 != C:
        Wp = np.zeros((K, ncc * 128), W.dtype)
        Wp[:, :C] = W
    else:
        Wp = W
    return np.ascontiguousarray(Wp.reshape(nkc, 128, ncc, 128).transpose(2, 1, 0, 3))


def fm(x):
    T, D = x.shape
    return np.ascontiguousarray(x.T.reshape(D // 128, 128, T))


def unfm(xT):
    n, p, T = xT.shape
    return np.ascontiguousarray(xT.reshape(n * p, T).T)


def vec_fm(v):
    sh = v.shape[:-1]
    D = v.shape[-1]
    a = v.reshape(sh + (D // 128, 128))
    a = np.moveaxis(a, -1, 0)
    return np.ascontiguousarray(a)


class TokCommon:
    def __init__(self, k, cfg, nmax):
        self.k = k
        self.cfg = cfg
        self.nmax = nmax
        self.ones = k.sbuf([128, 128], name="ones")
        I(k, "dve", "memset", [], [self.ones], self.ones[:], 1.0)
        self.sq = [k.sbuf([128, nmax], name=f"sq{i}") for i in range(2)]
        self.rstd = k.sbuf([128, nmax], name="rstd")
        self.banks = [k.psum([128, 512], name=f"bank{i}") for i in range(8)]
        self.wbuf = [k.sbuf([128, max(cfg.NKC, cfg.NKD) if hasattr(cfg, "NKD") else cfg.NKC, 128], name=f"wbuf{i}") for i in range(3)]
        self.wi = 0
        self.qs = ["sp", "act", "pool"]
        self.qi = 0

    def nextq(self):
        q = self.qs[self.qi % 2]
        self.qi += 1
        return q

    def load_w(self, src_ap, nk):
        k = self.k
        w = self.wbuf[self.wi % 3]
        self.wi += 1
        k.dma(self.nextq(), w[:, 0:nk, :], src_ap, writes=[w])
        return w

    def rms_stats(self, X, nk, n, D, eps=1e-6):
        k = self.k
        ss = self.banks[7]
        for kc in range(nk):
            sq = self.sq[kc % 2]
            I(k, "act", "activation", [X], [sq], out=sq[:, :n], in_=X[:, kc, :n], func=AF.Square)
            I(k, "pe", "matmul", [self.ones, sq], [ss], ss[:, :n], lhsT=self.ones[:], rhs=sq[:, :n],
              start=(kc == 0), stop=(kc == nk - 1))
        I(k, "act", "activation", [ss], [self.rstd], out=self.rstd[:, :n], in_=ss[:, :n], func=AF.Sqrt,
          scale=1.0 / D, bias=self.epst[:, 0:1])
        I(k, "dve", "reciprocal", [self.rstd], [self.rstd], out=self.rstd[:, :n], in_=self.rstd[:, :n])

    def norm_mod(self, X, O, nk, n, D, gs, sh):
        k = self.k
        self.rms_stats(X, nk, n, D)
        for kc in range(nk):
            I(k, "dve", "scalar_tensor_tensor", [X, self.rstd, self.prm], [O], out=O[:, kc, :n], in0=X[:, kc, :n],
              scalar=gs[:, kc:kc + 1], in1=self.rstd[:, :n], op0=ALU.mult, op1=ALU.mult)
            I(k, "act", "activation", [O, self.prm], [O], out=O[:, kc, :n], in_=O[:, kc, :n], func=AF.Identity,
              bias=sh[:, kc:kc + 1], scale=1.0)


def build_P0(D, ncc_core, L, R=3):
    k = KB()
    nkc = D // 128
    w = k.dram("w", [L, ncc_core, 128, nkc, 128], kind="ExternalInput")
    cT = k.dram("cT", [128, nkc, R], kind="ExternalInput")
    b = k.dram("b", [128, L, ncc_core], kind="ExternalInput")
    out = k.dram("out", [128, L, ncc_core, R], kind="ExternalOutput")
    cs = k.sbuf([128, nkc, R])
    sg = k.sbuf([128, nkc, R])
    bs = k.sbuf([128, L, ncc_core])
    os_ = k.sbuf([128, L, ncc_core, R])
    k.dma("sp", cs[:], cT[:], writes=[cs])
    k.dma("act", bs[:], b[:], writes=[bs])
    I(k, "act", "activation", [cs], [sg], out=sg[:], in_=cs[:], func=AF.Sigmoid)
    I(k, "dve", "tensor_tensor", [cs, sg], [cs], out=cs[:], in0=cs[:], in1=sg[:], op=ALU.mult)
    wb = [k.sbuf([128, nkc, 128], name=f"p0w{i}") for i in range(3)]
    banks = [k.psum([128, 512], name=f"p0b{i}") for i in range(2)]
    it = 0
    for l in range(L):
        for cc in range(ncc_core):
            wt = wb[it % 3]
            k.dma(["sp", "act"][it % 2], wt[:], w[l, cc], writes=[wt])
            ps = banks[it % 2]
            for kc in range(nkc):
                I(k, "pe", "matmul", [wt, cs], [ps], ps[:, 0:R], lhsT=wt[:, kc, :], rhs=cs[:, kc, :],
                  start=(kc == 0), stop=(kc == nkc - 1))
            I(k, "dve", "tensor_scalar", [ps, bs], [os_], out=os_[:, l, cc, :], in0=ps[:, 0:R],
              scalar1=bs[:, l, cc:cc + 1], scalar2=None, op0=ALU.add)
            it += 1
    k.dma("sp", out[:], os_[:], reads=[os_], writes=[out], is_output=True)
    k.finish()
    return k


def build_A(cfg):
    k = KB()
    NKC, NTOK, NCC, D = cfg.NKC, cfg.NTOK, cfg.NCC_IN, cfg.D
    xT = k.dram("xT", [NKC, 128, NTOK], kind="ExternalInput")
    w = k.dram("w", [NCC, 128, NKC, 128], kind="ExternalInput")
    prm = k.dram("prm", [128, 2, 3, NKC], kind="ExternalInput")
    zT = k.dram("zT", [NCC, 128, NTOK], kind="ExternalOutput")
    nmax = cfg.nt_a
    tc = TokCommon(k, cfg, nmax)
    tc.prm = k.sbuf([128, 2, 3, NKC], name="prm")
    tc.epst = k.sbuf([128, 1], name="eps")
    I(k, "dve", "memset", [], [tc.epst], tc.epst[:], 1e-6)
    k.dma("sp", tc.prm[:], prm[:], writes=[tc.prm])
    for ty in range(2):
        I(k, "dve", "scalar_tensor_tensor", [tc.prm], [tc.prm], out=tc.prm[:, ty, 1, :], in0=tc.prm[:, ty, 1, :],
          scalar=1.0, in1=tc.prm[:, ty, 2, :], op0=ALU.add, op1=ALU.mult)
    X = k.sbuf([128, NKC, nmax], name="X")
    zs = [k.sbuf([128, nmax], name=f"zs{i}") for i in range(3)]
    it = 0
    for (t0, n, ty) in cfg.tiles(cfg.nt_a):
        k.dma("sp", X[:, :, :n], xT[:, :, t0:t0 + n].rearrange("c p t -> p c t"), writes=[X])
        tc.norm_mod(X, X, NKC, n, D, tc.prm[:, ty, 1, :], tc.prm[:, ty, 0, :])
        for cc in range(NCC):
            wt = tc.load_w(w[cc], NKC)
            ps = tc.banks[it % 4]
            for kc in range(NKC):
                I(k, "pe", "matmul", [wt, X], [ps], ps[:, :n], lhsT=wt[:, kc, :], rhs=X[:, kc, :n],
                  start=(kc == 0), stop=(kc == NKC - 1))
            z = zs[it % 3]
            if it % 2 == 0:
                I(k, "act", "activation", [ps], [z], out=z[:, :n], in_=ps[:, :n], func=AF.Copy)
            else:
                I(k, "dve", "tensor_copy", [ps], [z], out=z[:, :n], in_=ps[:, :n])
            k.dma("pool", zT[cc, :, t0:t0 + n], z[:, :n], reads=[z], is_output=True)
            it += 1
    k.finish()
    return k


def build_C(cfg, final=False):
    k = KB()
    NKC, NTOK, D, E, NHC, NKD = cfg.NKC, cfg.NTOK, cfg.D, cfg.E, cfg.NHC, cfg.NKD
    NG = 4
    NR = NG + E
    xT = k.dram("xT", [NKC, 128, NTOK], kind="ExternalInput")
    yT = k.dram("yT", [NKC, 128, NTOK], kind="ExternalInput")
    w_out = k.dram("w_out", [NKC, 128, NKC, 128], kind="ExternalInput")
    prm = k.dram("prm", [128, 2, 6, NKC], kind="ExternalInput")
    wcf = k.dram("wcf", [128, NKC, NR], kind="ExternalInput")
    bcf = k.dram("bcf", [128, NR], kind="ExternalInput")
    w_gu = k.dram("w_gu", [E, NHC, 128, NKC, 128], kind="ExternalInput")
    w_dn = k.dram("w_dn", [NKC, 128, NKD, 128], kind="ExternalInput")
    sel = k.dram("sel", [E, E, 128], kind="ExternalInput")
    idn = k.dram("idn", [128, 128], kind="ExternalInput")
    xo = k.dram("xo", [NKC, 128, NTOK], kind="ExternalOutput")
    nmax = cfg.nt_c
    tc = TokCommon(k, cfg, nmax)
    tc.prm = k.sbuf([128, 2, 6, NKC], name="prm")
    tc.epst = k.sbuf([128, 1], name="eps")
    I(k, "dve", "memset", [], [tc.epst], tc.epst[:], 1e-6)
    k.dma("sp", tc.prm[:], prm[:], writes=[tc.prm])
    WCF = k.sbuf([128, NKC, NR], name="WCF")
    BCF = k.sbuf([128, NR], name="BCF")
    SEL = k.sbuf([E, E, 128], name="SEL")
    IDN = k.sbuf([128, 128], name="IDN")
    k.dma("act", WCF[:], wcf[:], writes=[WCF])
    k.dma("act", BCF[:], bcf[:], writes=[BCF])
    k.dma("pool", SEL[:], sel[:], writes=[SEL])
    k.dma("pool", IDN[:], idn[:], writes=[IDN])
    for ty in range(2):
        I(k, "dve", "scalar_tensor_tensor", [tc.prm], [tc.prm], out=tc.prm[:, ty, 2, :], in0=tc.prm[:, ty, 2, :],
          scalar=1.0, in1=tc.prm[:, ty, 4, :], op0=ALU.add, op1=ALU.mult)
    X = k.sbuf([128, NKC, nmax], name="X")
    Y = k.sbuf([128, NKC, nmax], name="Y")
    AT = k.sbuf([128, NKD, nmax], name="AT")
    GT = k.sbuf([E, nmax], name="GT")
    t1 = [k.sbuf([128, nmax], name=f"t1_{i}") for i in range(2)]
    def st(shape, nm):
        return k.sbuf(shape, name=nm)
    Lg = st([128, NR], "Lg"); gmax = st([128, 1], "gmax"); ngmax = st([128, 1], "ngmax")
    ohg = st([128, NG, 1], "ohg"); eg = st([128, NG], "eg"); sume = st([128, 1], "sume"); gprob = st([128, 1], "gprob")
    prod = st([128, NG, 4], "prod"); lsel = st([128, 4], "lsel"); m1 = st([128, 1], "m1"); mk1 = st([128, 4], "mk1")
    le2 = st([128, 4], "le2"); m2 = st([128, 1], "m2"); mk2 = st([128, 4], "mk2"); dd = st([128, 1], "dd")
    e2 = st([128, 1], "e2"); w1 = st([128, 1], "w1"); w2 = st([128, 1], "w2"); gin = st([128, 1, 4], "gin")
    gates = st([128, NG, 4], "gates")
    HB = tc.banks[0:4]
    AB = tc.banks[4:6]
    GB = tc.banks[6]
    RB = tc.banks[7]
    ai = 0
    for (t0, n, ty) in cfg.tiles(cfg.nt_c):
        P = tc.prm
        k.dma("sp", X[:, :, :n], xT[:, :, t0:t0 + n].rearrange("c p t -> p c t"), writes=[X])
        k.dma("act", Y[:, :, :n], yT[:, :, t0:t0 + n].rearrange("c p t -> p c t"), writes=[Y])
        for dc in range(NKC):
            wt = tc.load_w(w_out[dc], NKC)
            ps = AB[ai % 2]; ai += 1
            for kc in range(NKC):
                I(k, "pe", "matmul", [wt, Y], [ps], ps[:, :n], lhsT=wt[:, kc, :], rhs=Y[:, kc, :n],
                  start=(kc == 0), stop=(kc == NKC - 1))
            I(k, "dve", "scalar_tensor_tensor", [ps, X, P], [X], out=X[:, dc, :n], in0=ps[:, :n],
              scalar=P[:, ty, 0, dc:dc + 1], in1=X[:, dc, :n], op0=ALU.mult, op1=ALU.add)
        tc.norm_mod(X, Y, NKC, n, D, P[:, ty, 2, :], P[:, ty, 1, :])
        for tb in range(-(-n // 128)):
            nb = min(128, n - tb * 128)
            for kc in range(NKC):
                I(k, "pe", "matmul", [Y, WCF], [RB], RB[:nb, 0:NR], lhsT=Y[:, kc, tb * 128:tb * 128 + nb],
                  rhs=WCF[:, kc, :], start=(kc == 0), stop=(kc == NKC - 1))
            V = lambda *a, **kw: I(k, "dve", *a, **kw)
            V("tensor_tensor", [RB, BCF], [Lg], out=Lg[:nb, :], in0=RB[:nb, 0:NR], in1=BCF[:nb, :], op=ALU.add)
            V("tensor_reduce", [Lg], [gmax], out=gmax[:nb, :], in_=Lg[:nb, 0:NG], op=ALU.max, axis=AX.X)
            V("tensor_scalar", [gmax], [ngmax], out=ngmax[:nb, :], in0=gmax[:nb, :], scalar1=-1.0, scalar2=None, op0=ALU.mult)
            V("tensor_scalar", [Lg, gmax], [ohg], out=ohg[:nb, :, 0], in0=Lg[:nb, 0:NG], scalar1=gmax[:nb, 0:1],
              scalar2=None, op0=ALU.is_equal)
            I(k, "act", "activation", [Lg, ngmax], [eg, sume], out=eg[:nb, :], in_=Lg[:nb, 0:NG], func=AF.Exp,
              bias=ngmax[:nb, 0:1], scale=1.0, accum_out=sume[:nb, 0:1])
            V("reciprocal", [sume], [gprob], out=gprob[:nb, :], in_=sume[:nb, :])
            V("tensor_tensor", [Lg, ohg], [prod], out=prod[:nb], in0=Lg[:nb, NG:NR].rearrange("p (g e) -> p g e", g=NG),
              in1=ohg[:nb].to_broadcast([nb, NG, 4]), op=ALU.mult)
            V("tensor_reduce", [prod], [lsel], out=lsel[:nb, :], in_=prod[:nb].rearrange("p g e -> p e g"), op=ALU.add, axis=AX.X)
            V("tensor_reduce", [lsel], [m1], out=m1[:nb, :], in_=lsel[:nb, :], op=ALU.max, axis=AX.X)
            V("tensor_scalar", [lsel, m1], [mk1], out=mk1[:nb, :], in0=lsel[:nb, :], scalar1=m1[:nb, 0:1], scalar2=None, op0=ALU.is_equal)
            V("scalar_tensor_tensor", [mk1, lsel], [le2], out=le2[:nb, :], in0=mk1[:nb, :], scalar=-1e30, in1=lsel[:nb, :],
              op0=ALU.mult, op1=ALU.add)
            V("tensor_reduce", [le2], [m2], out=m2[:nb, :], in_=le2[:nb, :], op=ALU.max, axis=AX.X)
            V("tensor_scalar", [le2, m2], [mk2], out=mk2[:nb, :], in0=le2[:nb, :], scalar1=m2[:nb, 0:1], scalar2=None, op0=ALU.is_equal)
            V("tensor_tensor", [m2, m1], [dd], out=dd[:nb, :], in0=m2[:nb, :], in1=m1[:nb, :], op=ALU.subtract)
            I(k, "act", "activation", [dd], [e2], out=e2[:nb, :], in_=dd[:nb, :], func=AF.Exp)
            V("tensor_scalar", [e2], [w1], out=w1[:nb, :], in0=e2[:nb, :], scalar1=1.0, scalar2=None, op0=ALU.add)
            V("reciprocal", [w1], [w1], out=w1[:nb, :], in_=w1[:nb, :])
            V("tensor_tensor", [e2, w1], [w2], out=w2[:nb, :], in0=e2[:nb, :], in1=w1[:nb, :], op=ALU.mult)
            V("tensor_scalar", [mk1, w1], [gin], out=gin[:nb, 0, :], in0=mk1[:nb, :], scalar1=w1[:nb, 0:1], scalar2=None, op0=ALU.mult)
            V("scalar_tensor_tensor", [mk2, w2, gin], [gin], out=gin[:nb, 0, :], in0=mk2[:nb, :], scalar=w2[:nb, 0:1],
              in1=gin[:nb, 0, :], op0=ALU.mult, op1=ALU.add)
            V("tensor_scalar", [gin, gprob], [gin], out=gin[:nb, 0, :], in0=gin[:nb, 0, :], scalar1=gprob[:nb, 0:1], scalar2=None, op0=ALU.mult)
            V("tensor_tensor", [ohg, gin], [gates], out=gates[:nb], in0=ohg[:nb].to_broadcast([nb, NG, 4]),
              in1=gin[:nb].to_broadcast([nb, NG, 4]), op=ALU.mult)
            I(k, "pe", "transpose", [gates, IDN], [RB], RB[:E, 256:256 + nb], gates[:nb].rearrange("p g e -> p (g e)"), IDN[:nb, :nb])
            V("tensor_copy", [RB], [GT], out=GT[:, tb * 128:tb * 128 + nb], in_=RB[:E, 256:256 + nb])
        for e in range(E):
            for hc in range(NHC):
                wt = tc.load_w(w_gu[e, hc], NKC)
                for kc in range(NKC):
                    I(k, "pe", "matmul", [wt, Y], [HB[hc]], HB[hc][:, :n], lhsT=wt[:, kc, :], rhs=Y[:, kc, :n],
                      start=(kc == 0), stop=(kc == NKC - 1))
            I(k, "pe", "matmul", [SEL, GT], [GB], GB[:, :n], lhsT=SEL[:, e, :], rhs=GT[:, :n], start=True, stop=True)
            for j in range(NHC // 2):
                I(k, "act", "activation", [HB[j]], [t1[j]], out=t1[j][:, :n], in_=HB[j][:, :n], func=AF.Silu)
                I(k, "dve", "tensor_tensor", [t1[j], HB[NHC // 2 + j]], [t1[j]], out=t1[j][:, :n], in0=t1[j][:, :n],
                  in1=HB[NHC // 2 + j][:, :n], op=ALU.mult)
                I(k, "dve", "tensor_tensor", [t1[j], GB], [AT], out=AT[:, e * (NHC // 2) + j, :n], in0=t1[j][:, :n],
                  in1=GB[:, :n], op=ALU.mult)
        for dc in range(NKC):
            wt = tc.load_w(w_dn[dc], NKD)
            ps = AB[ai % 2]; ai += 1
            for kc in range(NKD):
                I(k, "pe", "matmul", [wt, AT], [ps], ps[:, :n], lhsT=wt[:, kc, :], rhs=AT[:, kc, :n],
                  start=(kc == 0), stop=(kc == NKD - 1))
            I(k, "dve", "scalar_tensor_tensor", [ps, X, P], [X], out=X[:, dc, :n], in0=ps[:, :n],
              scalar=P[:, ty, 3, dc:dc + 1], in1=X[:, dc, :n], op0=ALU.mult, op1=ALU.add)
        if final:
            tc.rms_stats(X, NKC, n, D)
            for kc in range(NKC):
                I(k, "dve", "scalar_tensor_tensor", [X, tc.rstd, P], [X], out=X[:, kc, :n], in0=X[:, kc, :n],
                  scalar=P[:, ty, 5, kc:kc + 1], in1=tc.rstd[:, :n], op0=ALU.mult, op1=ALU.mult)
        k.dma("pool", xo[:, :, t0:t0 + n].rearrange("c p t -> p c t"), X[:, :, :n], reads=[X], is_output=True)
    k.finish()
    return k


def seq_segments(Tc, Tl, SEG):
    segs = [(0, Tc, 0, Tc)]
    for s in range(Tc, Tc + Tl, SEG):
        segs.append((s, min(s + SEG, Tc + Tl), Tc, Tc + Tl))
    return segs


def gelu_tanh(k, out, x, tmp, n):
    I(k, "dve", "tensor_tensor", [x], [tmp], out=tmp[:, :n], in0=x[:, :n], in1=x[:, :n], op=ALU.mult)
    I(k, "dve", "tensor_scalar", [tmp], [tmp], out=tmp[:, :n], in0=tmp[:, :n], scalar1=0.044715, scalar2=1.0,
      op0=ALU.mult, op1=ALU.add)
    I(k, "dve", "tensor_tensor", [tmp, x], [tmp], out=tmp[:, :n], in0=tmp[:, :n], in1=x[:, :n], op=ALU.mult)
    I(k, "act", "activation", [tmp], [tmp], out=tmp[:, :n], in_=tmp[:, :n], func=AF.Sigmoid, scale=1.5957691216057308)
    I(k, "dve", "tensor_tensor", [tmp, x], [out], out=out[:, :n], in0=tmp[:, :n], in1=x[:, :n], op=ALU.mult)


def build_LRU(Tc, Tl, SEG, NB=2):
    k = KB()
    T = Tc + Tl
    zx = k.dram("zx", [NB, 128, T], kind="ExternalInput")
    zy = k.dram("zy", [NB, 128, T], kind="ExternalInput")
    prm = k.dram("prm", [128, NB, 11], kind="ExternalInput")
    wa = k.dram("wa", [128, NB, 2, 128], kind="ExternalInput")
    wx = k.dram("wx", [128, NB, 2, 128], kind="ExternalInput")
    yo = k.dram("yo", [NB, 128, T], kind="ExternalOutput")
    P = k.sbuf([128, NB, 11], name="P"); WA = k.sbuf([128, NB, 2, 128], name="WA"); WX = k.sbuf([128, NB, 2, 128], name="WX")
    k.dma("sp", P[:], prm[:], writes=[P]); k.dma("act", WA[:], wa[:], writes=[WA]); k.dma("pool", WX[:], wx[:], writes=[WX])
    one = k.sbuf([128, 1], name="one")
    I(k, "dve", "memset", [], [one], one[:], 1.0)
    C8 = k.sbuf([128, NB, 2], name="C8"); C16 = k.sbuf([128, NB, 2], name="C16")
    I(k, "act", "activation", [P], [C8], out=C8[:], in_=P[:, :, 9:11], func=AF.Exp, scale=-1.0)
    I(k, "act", "activation", [C8, one], [C8], out=C8[:], in_=C8[:], func=AF.Ln, bias=one[:, 0:1], scale=1.0)
    I(k, "dve", "tensor_scalar", [C8], [C16], out=C16[:], in0=C8[:], scalar1=-16.0, scalar2=None, op0=ALU.mult)
    I(k, "dve", "tensor_scalar", [C8], [C8], out=C8[:], in0=C8[:], scalar1=-8.0, scalar2=None, op0=ALU.mult)
    OF = k.sbuf([128, T], name="OF")
    XH = k.sbuf([128, SEG + 3], name="XH"); XC = k.sbuf([128, SEG], name="XC")
    Rr = k.sbuf([128, SEG], name="Rr"); Ii = k.sbuf([128, SEG], name="Ii"); A2 = k.sbuf([128, SEG], name="A2")
    YG = k.sbuf([128, SEG], name="YG"); OB = k.sbuf([128, SEG], name="OB"); TM = k.sbuf([128, SEG], name="TM")
    hst = k.sbuf([128, 1], name="hst")
    banks = [k.psum([128, 512], name=f"lb{i}") for i in range(4)]
    segs = seq_segments(Tc, Tl, SEG)
    bi = 0

    def prep(blk, d, seg):
        nonlocal bi
        s0, s1, q0, q1 = seg
        n = s1 - s0
        lo, hi = max(s0 - 2, q0), min(s1 + 1, q1)
        I(k, "pool", "memset", [], [XH], XH[:, :n + 3], 0.0)
        k.dma("sp", XH[:, lo - (s0 - 2):hi - (s0 - 2)], zx[blk, :, lo:hi], writes=[XH])
        I(k, "dve", "tensor_scalar", [XH, P], [XC], out=XC[:, :n], in0=XH[:, 0:n], scalar1=P[:, blk, 0:1],
          scalar2=P[:, blk, 4:5], op0=ALU.mult, op1=ALU.add)
        for j in range(1, 4):
            I(k, "dve", "scalar_tensor_tensor", [XH, P, XC], [XC], out=XC[:, :n], in0=XH[:, j:j + n],
              scalar=P[:, blk, j:j + 1], in1=XC[:, :n], op0=ALU.mult, op1=ALU.add)
        for c0 in range(0, n, 512):
            cn = min(512, n - c0)
            pa = banks[bi % 4]; px = banks[(bi + 1) % 4]; bi += 2
            I(k, "pe", "matmul", [WA, XC], [pa], pa[:, :cn], lhsT=WA[:, blk, d, :], rhs=XC[:, c0:c0 + cn], start=True, stop=True)
            I(k, "pe", "matmul", [WX, XC], [px], px[:, :cn], lhsT=WX[:, blk, d, :], rhs=XC[:, c0:c0 + cn], start=True, stop=True)
            I(k, "act", "activation", [pa, P], [Rr], out=Rr[:, c0:c0 + cn], in_=pa[:, :cn], func=AF.Sigmoid,
              bias=P[:, blk, 5 + d:6 + d], scale=1.0)
            I(k, "act", "activation", [px, P], [Ii], out=Ii[:, c0:c0 + cn], in_=px[:, :cn], func=AF.Sigmoid,
              bias=P[:, blk, 7 + d:8 + d], scale=1.0)
        I(k, "act", "activation", [Rr, C16], [A2], out=A2[:, :n], in_=Rr[:, :n], func=AF.Exp, scale=C16[:, blk, d:d + 1])
        I(k, "act", "activation", [Rr, C8], [Rr], out=Rr[:, :n], in_=Rr[:, :n], func=AF.Exp, scale=C8[:, blk, d:d + 1])
        I(k, "act", "activation", [A2, one], [A2], out=A2[:, :n], in_=A2[:, :n], func=AF.Sqrt, scale=-1.0, bias=one[:, 0:1])
        I(k, "dve", "tensor_tensor", [A2, Ii], [A2], out=A2[:, :n], in0=A2[:, :n], in1=Ii[:, :n], op=ALU.mult)
        I(k, "dve", "tensor_tensor", [A2, XC], [A2], out=A2[:, :n], in0=A2[:, :n], in1=XC[:, :n], op=ALU.mult)
        return n

    for blk in range(NB):
        I(k, "dve", "memset", [], [hst], hst[:], 0.0)
        for seg in segs:
            n = prep(blk, 0, seg)
            s0 = seg[0]
            I(k, "dve", "tensor_tensor_scan", [Rr, A2, hst], [OF], out=OF[:, s0:s0 + n], data0=Rr[:, :n], data1=A2[:, :n],
              initial=hst[:, 0:1], op0=ALU.mult, op1=ALU.add)
            I(k, "dve", "tensor_copy", [OF], [hst], out=hst[:], in_=OF[:, s0 + n - 1:s0 + n])
        I(k, "dve", "memset", [], [hst], hst[:], 0.0)
        for seg in [segs[0]] + segs[1:][::-1]:
            n = prep(blk, 1, seg)
            s0 = seg[0]
            k.dma("act", YG[:, :n], zy[blk, :, s0:s0 + n], writes=[YG])
            I(k, "dve", "tensor_tensor_scan", [Rr, A2, hst], [OB], out=OB[:, :n][:, ::-1], data0=Rr[:, :n][:, ::-1],
              data1=A2[:, :n][:, ::-1], initial=hst[:, 0:1], op0=ALU.mult, op1=ALU.add)
            I(k, "dve", "tensor_copy", [OB], [hst], out=hst[:], in_=OB[:, 0:1])
            gelu_tanh(k, Ii, YG, TM, n)
            I(k, "dve", "tensor_tensor", [OB, OF], [OB], out=OB[:, :n], in0=OB[:, :n], in1=OF[:, s0:s0 + n], op=ALU.add)
            I(k, "dve", "tensor_tensor", [OB, Ii], [OB], out=OB[:, :n], in0=OB[:, :n], in1=Ii[:, :n], op=ALU.mult)
            k.dma("pool", yo[blk, :, s0:s0 + n], OB[:, :n], reads=[OB], is_output=True)
    k.finish()
    return k


POOL_WINDOWS = (2, 4, 8, 16)


def build_POOL(groups):
    k = KB()
    NG = len(groups)
    zin = [k.dram(f"zp{i}", [8, 128, nl, lw + 16], kind="ExternalInput") for i, (nl, lw) in enumerate(groups)]
    inv = [k.dram(f"inv{i}", [128, 4, lw], kind="ExternalInput") for i, (nl, lw) in enumerate(groups)]
    lin = k.dram("lin", [128, 4, 2, 256], kind="ExternalInput")
    scl = k.dram("scl", [128, 8], kind="ExternalInput")
    yout = [k.dram(f"yp{i}", [8, 128, nl, lw], kind="ExternalOutput") for i, (nl, lw) in enumerate(groups)]
    LIN = k.sbuf([128, 4, 2, 256], name="LIN"); SCL = k.sbuf([128, 8], name="SCL")
    k.dma("sp", LIN[:], lin[:], writes=[LIN]); k.dma("act", SCL[:], scl[:], writes=[SCL])
    banks = [k.psum([128, 512], name=f"pb{i}") for i in range(4)]
    bi = 0
    for gi_, (NL, LW) in enumerate(groups):
        LP = LW + 16
        INV = k.sbuf([128, 4, 1, LW], name=f"INV{gi_}")
        k.dma("act", INV[:, :, 0, :], inv[gi_][:], writes=[INV])
        XH = [k.sbuf([128, NL, LP], name=f"XH{gi_}_{i}") for i in range(2)]
        SA = k.sbuf([128, NL, LP], name=f"SA{gi_}"); SB = k.sbuf([128, NL, LP], name=f"SB{gi_}")
        M = [k.sbuf([128, NL, LW], name=f"M{gi_}_{i}") for i in range(2)]
        O = [k.sbuf([128, 512], name=f"O{gi_}_{i}") for i in range(2)]
        ntok = NL * LW
        for g, w in enumerate(POOL_WINDOWS):
            for ic in range(2):
                cb = 2 * g + ic
                k.dma(["sp", "act"][ic], XH[ic][:], zin[gi_][cb], writes=[XH[ic]])
                src = XH[ic]; L = LP; sh = 1
                bufs = [SA, SB]; bsel = 0
                while sh < w:
                    dst = bufs[bsel]; bsel ^= 1
                    I(k, "dve", "tensor_tensor", [src], [dst], out=dst[:, :, 0:L - sh], in0=src[:, :, 0:L - sh],
                      in1=src[:, :, sh:L], op=ALU.add)
                    src = dst; L -= sh; sh *= 2
                o = 8 - w // 2
                I(k, "dve", "tensor_tensor", [src, INV], [M[ic]], out=M[ic][:], in0=src[:, :, o:o + LW],
                  in1=INV[:, g].to_broadcast([128, NL, LW]), op=ALU.mult)
                I(k, "dve", "tensor_tensor", [M[ic], XH[ic]], [M[ic]], out=M[ic][:], in0=M[ic][:], in1=XH[ic][:, :, 8:8 + LW],
                  op=ALU.subtract)
            for oc in range(2):
                for c0 in range(0, ntok, 512):
                    cn = min(512, ntok - c0)
                    ps = banks[bi % 4]; ot = O[bi % 2]; bi += 1
                    for ic in range(2):
                        I(k, "pe", "matmul", [LIN, M[ic]], [ps], ps[:, :cn], lhsT=LIN[:, g, ic, oc * 128:(oc + 1) * 128],
                          rhs=M[ic][:].rearrange("p l t -> p (l t)")[:, c0:c0 + cn], start=(ic == 0), stop=(ic == 1))
                    I(k, "act", "activation", [ps, SCL], [ot], out=ot[:, :cn], in_=ps[:, :cn], func=AF.Copy,
                      scale=SCL[:, 2 * g + oc:2 * g + oc + 1])
                    k.dma("pool", yout[gi_][2 * g + oc].rearrange("p l t -> p (l t)")[:, c0:c0 + cn], ot[:, :cn], reads=[ot], is_output=True)
    k.finish()
    return k


def pool_invcount(n, positions):
    out = np.zeros((4, len(positions)), np.float32)
    for g, w in enumerate(POOL_WINDOWS):
        t = np.asarray(positions)
        lo = np.clip(t - w // 2, 0, n); hi = np.clip(t + w // 2, 0, n)
        out[g] = 1.0 / (hi - lo)
    return out


HG_C = 32


def build_HG(Tc, Tl, SEG, NH=2, L=4):
    k = KB()
    T = Tc + Tl
    C = HG_C
    zd = [k.dram(f"zd{d}", [NH, 3, 128, T], kind="ExternalInput") for d in range(2)]
    zg = k.dram("zg", [NH, 128, T], kind="ExternalInput")
    lbp = k.dram("lbp", [128, NH, 2, L], kind="ExternalInput")
    lmask = k.dram("lmask", [128, L], kind="ExternalInput")
    gn = k.dram("gn", [128, 1], kind="ExternalInput")
    msk = k.dram("msk", [C, C], kind="ExternalInput")
    idn = k.dram("idn", [128, 128], kind="ExternalInput")
    yo = k.dram("yo", [NH, 128, T], kind="ExternalOutput")
    LBP = k.sbuf([128, NH, 2, L], name="LBP"); LM = k.sbuf([128, 1, 1, L], name="LM"); GN = k.sbuf([128, 1], name="GN")
    MSK = k.sbuf([C, C], name="MSK"); IDN = k.sbuf([128, 128], name="IDN")
    k.dma("sp", LBP[:], lbp[:], writes=[LBP]); k.dma("sp", LM[:, 0, 0, :], lmask[:], writes=[LM]); k.dma("sp", GN[:], gn[:], writes=[GN])
    k.dma("act", MSK[:], msk[:], writes=[MSK]); k.dma("act", IDN[:], idn[:], writes=[IDN])
    ones = k.sbuf([128, max(SEG, 128)], name="ones")
    I(k, "dve", "memset", [], [ones], ones[:], 1.0)
    LB = k.sbuf([128, NH, 2], name="LB"); OML = k.sbuf([128, NH, 2], name="OML")
    mx = k.sbuf([128, NH, 2], name="mx"); sm = k.sbuf([128, NH, 2], name="sm")
    EX = k.sbuf([128, NH, 2, L], name="EX")
    I(k, "dve", "tensor_reduce", [LBP], [mx], out=mx[:], in_=LBP[:], op=ALU.max, axis=AX.X)
    I(k, "dve", "tensor_tensor", [LBP, mx], [EX], out=EX[:], in0=LBP[:], in1=mx[:].unsqueeze(3).to_broadcast([128, NH, 2, L]), op=ALU.subtract)
    I(k, "act", "activation", [EX], [EX], out=EX[:], in_=EX[:], func=AF.Exp)
    I(k, "dve", "tensor_reduce", [EX], [sm], out=sm[:], in_=EX[:], op=ALU.add, axis=AX.X)
    I(k, "dve", "tensor_tensor", [EX, LM], [EX], out=EX[:], in0=EX[:], in1=LM[:].to_broadcast([128, NH, 2, L]), op=ALU.mult)
    I(k, "dve", "tensor_reduce", [EX], [LB], out=LB[:], in_=EX[:], op=ALU.add, axis=AX.X)
    I(k, "dve", "reciprocal", [sm], [sm], out=sm[:], in_=sm[:])
    I(k, "dve", "tensor_tensor", [LB, sm], [LB], out=LB[:], in0=LB[:], in1=sm[:], op=ALU.mult)
    I(k, "dve", "tensor_scalar", [LB], [OML], out=OML[:], in0=LB[:], scalar1=-1.0, scalar2=1.0, op0=ALU.mult, op1=ALU.add)

    NCH = SEG // C
    OD = [k.sbuf([128, T], name=f"OD{d}") for d in range(2)]
    ZQ = k.sbuf([128, SEG], name="ZQ"); ZF = k.sbuf([128, SEG], name="ZF"); ZV = k.sbuf([128, SEG], name="ZV")
    KK = k.sbuf([128, SEG], name="KK"); CUM = k.sbuf([128, SEG], name="CUM"); DD = k.sbuf([128, SEG], name="DD")
    E2 = k.sbuf([128, SEG], name="E2")
    BASE = k.sbuf([128, NCH], name="BASE"); EM = k.sbuf([128, NCH], name="EM"); ETOT = k.sbuf([128, NCH], name="ETOT")
    S = k.sbuf([128, 128], name="S"); SP = [k.sbuf([128, 128], name=f"SP{i}") for i in range(2)]
    KV = [k.sbuf([C, 256], name=f"KV{i}") for i in range(2)]
    SCM = [k.sbuf([C, C], name=f"SCM{i}") for i in range(2)]
    cst = k.sbuf([128, 1], name="cst")
    TP = [k.psum([128, 512], name=f"TP{i}") for i in range(2)]
    SC = [k.psum([128, 512], name=f"SC{i}") for i in range(2)]
    DP = [k.psum([128, 512], name=f"DP{i}") for i in range(2)]
    OP = [k.psum([128, 512], name=f"OP{i}") for i in range(2)]
    segs = seq_segments(Tc, Tl, SEG)
    ci = 0
    opi = 0
    for h in range(NH):
        for d in range(2):
            I(k, "dve", "memset", [], [S], S[:], 0.0)
            I(k, "dve", "memset", [], [cst], cst[:], 0.0)
            for (s0, s1, q0, q1) in segs:
                n = s1 - s0
                nch = n // C
                k.dma("sp", ZQ[:, :n], zd[d][h, 0, :, s0:s1], writes=[ZQ])
                k.dma("act", ZF[:, :n], zd[d][h, 1, :, s0:s1], writes=[ZF])
                k.dma("pool", ZV[:, :n], zd[d][h, 2, :, s0:s1], writes=[ZV])
                I(k, "act", "activation", [ZQ], [ZQ], out=ZQ[:, :n], in_=ZQ[:, :n], func=AF.Silu)
                I(k, "act", "activation", [ZF], [ZF], out=ZF[:, :n], in_=ZF[:, :n], func=AF.Sigmoid)
                I(k, "dve", "tensor_scalar", [ZF, OML, LB], [ZF], out=ZF[:, :n], in0=ZF[:, :n], scalar1=OML[:, h, d:d + 1],
                  scalar2=LB[:, h, d:d + 1], op0=ALU.mult, op1=ALU.add)
                I(k, "dve", "tensor_scalar", [ZF], [KK], out=KK[:, :n], in0=ZF[:, :n], scalar1=-1.0, scalar2=1.0, op0=ALU.mult, op1=ALU.add)
                I(k, "act", "activation", [ZF], [ZF], out=ZF[:, :n], in_=ZF[:, :n], func=AF.Ln)
                I(k, "dve", "tensor_tensor_scan", [ones, ZF], [CUM], out=CUM[:, :n], data0=ones[:, :n], data1=ZF[:, :n],
                  initial=0.0, op0=ALU.mult, op1=ALU.add)
                C3 = CUM[:, :n].rearrange("p (c t) -> p c t", t=C)
                D3 = DD[:, :n].rearrange("p (c t) -> p c t", t=C)
                I(k, "dve", "memset", [], [BASE], BASE[:, 0:1], 0.0)
                if nch > 1:
                    I(k, "dve", "tensor_copy", [CUM], [BASE], out=BASE[:, 1:nch], in_=C3[:, 0:nch - 1, C - 1])
                I(k, "dve", "tensor_tensor", [CUM, BASE], [EM], out=EM[:, :nch], in0=C3[:, :, C // 2 - 1], in1=BASE[:, :nch], op=ALU.subtract)
                I(k, "dve", "tensor_tensor", [CUM, BASE], [ETOT], out=ETOT[:, :nch], in0=C3[:, :, C - 1], in1=BASE[:, :nch], op=ALU.subtract)
                I(k, "act", "activation", [EM], [EM], out=EM[:, :nch], in_=EM[:, :nch], func=AF.Exp)
                I(k, "act", "activation", [ETOT], [ETOT], out=ETOT[:, :nch], in_=ETOT[:, :nch], func=AF.Exp)
                I(k, "dve", "tensor_tensor", [CUM], [DD], out=D3, in0=C3, in1=C3[:, :, C // 2 - 1:C // 2].to_broadcast([128, nch, C]), op=ALU.subtract)
                I(k, "act", "activation", [DD], [E2], out=E2[:, :n], in_=DD[:, :n], func=AF.Exp, scale=-1.0)
                I(k, "act", "activation", [DD], [DD], out=DD[:, :n], in_=DD[:, :n], func=AF.Exp)
                I(k, "dve", "scalar_tensor_tensor", [ZQ, DD], [ZQ], out=ZQ[:, :n], in0=ZQ[:, :n], scalar=float(128 ** -0.5), in1=DD[:, :n],
                  op0=ALU.mult, op1=ALU.mult)
                I(k, "dve", "tensor_tensor", [KK, E2], [KK], out=KK[:, :n], in0=KK[:, :n], in1=E2[:, :n], op=ALU.mult)
                for c in range(nch):
                    cs = slice(c * C, (c + 1) * C)
                    tp = TP[ci % 2]; sc = SC[ci % 2]; dp = DP[ci % 2]; kv = KV[ci % 2]; scm = SCM[ci % 2]; sp = SP[ci % 2]
                    if c % 16 == 0:
                        op_ = OP[opi % 2]; opi += 1
                    oc = (c % 16) * C
                    ci += 1
                    I(k, "pe", "transpose", [KK, IDN], [tp], tp[:C, 0:128], KK[:, cs], IDN[:])
                    I(k, "pe", "transpose", [ZV, IDN], [tp], tp[:C, 128:256], ZV[:, cs], IDN[:])
                    I(k, "act", "activation", [tp], [kv], out=kv[:], in_=tp[:C, 0:256], func=AF.Copy)
                    I(k, "pe", "matmul", [KK, ZQ], [sc], sc[:C, 0:C], lhsT=KK[:, cs], rhs=ZQ[:, cs], start=True, stop=True)
                    I(k, "dve", "tensor_tensor", [sc, MSK], [scm], out=scm[:], in0=sc[:C, 0:C], in1=MSK[:], op=ALU.mult)
                    I(k, "pe", "matmul", [kv], [dp], dp[:, 0:128], lhsT=kv[:, 0:128], rhs=kv[:, 128:256], start=True, stop=True)
                    I(k, "dve", "tensor_scalar", [S, EM], [sp], out=sp[:], in0=S[:], scalar1=EM[:, c:c + 1], scalar2=None, op0=ALU.mult)
                    I(k, "pe", "matmul", [kv, scm], [op_], op_[:, oc:oc + C], lhsT=kv[:, 128:256], rhs=scm[:], start=True, stop=False)
                    I(k, "pe", "matmul", [sp, ZQ], [op_], op_[:, oc:oc + C], lhsT=sp[:], rhs=ZQ[:, cs], start=False, stop=True)
                    I(k, "dve", "tensor_scalar", [S, ETOT], [S], out=S[:], in0=S[:], scalar1=ETOT[:, c:c + 1], scalar2=None, op0=ALU.mult)
                    I(k, "dve", "scalar_tensor_tensor", [dp, DD, S], [S], out=S[:], in0=dp[:, 0:128], scalar=DD[:, (c + 1) * C - 1:(c + 1) * C],
                      in1=S[:], op0=ALU.mult, op1=ALU.add)
                    if c % 16 == 15 or c == nch - 1:
                        c0 = (c // 16) * 16 * C
                        w = (c % 16 + 1) * C
                        I(k, "act", "activation", [op_], [OD[d]], out=OD[d][:, s0 + c0:s0 + c0 + w], in_=op_[:, 0:w], func=AF.Copy)
        ZG = ZQ; OS = KK; SQ = CUM; RS = DD; TMP = E2
        for (s0, s1, q0, q1) in segs:
            n = s1 - s0
            f0 = q0 + (q1 - s1); f1 = q0 + (q1 - s0)
            k.dma("sp", ZG[:, :n], zg[h, :, s0:s1], writes=[ZG])
            I(k, "dve", "tensor_tensor", [OD[0], OD[1]], [OS], out=OS[:, :n], in0=OD[0][:, s0:s1], in1=OD[1][:, f0:f1][:, ::-1], op=ALU.add)
            I(k, "act", "activation", [OS], [SQ], out=SQ[:, :n], in_=OS[:, :n], func=AF.Square)
            for c0 in range(0, n, 512):
                cn = min(512, n - c0)
                ps = OP[opi % 2]; opi += 1
                I(k, "pe", "matmul", [ones, SQ], [ps], ps[:, :cn], lhsT=ones[:, 0:128], rhs=SQ[:, c0:c0 + cn], start=True, stop=True)
                I(k, "dve", "tensor_scalar", [ps], [RS], out=RS[:, c0:c0 + cn], in0=ps[:, :cn], scalar1=1.0 / 128, scalar2=1e-6, op0=ALU.mult, op1=ALU.add)
            I(k, "act", "activation", [RS], [RS], out=RS[:, :n], in_=RS[:, :n], func=AF.Sqrt)
            I(k, "dve", "reciprocal", [RS], [RS], out=RS[:, :n], in_=RS[:, :n])
            I(k, "dve", "scalar_tensor_tensor", [OS, GN, RS], [OS], out=OS[:, :n], in0=OS[:, :n], scalar=GN[:, 0:1], in1=RS[:, :n],
              op0=ALU.mult, op1=ALU.mult)
            I(k, "act", "activation", [ZG], [ZG], out=ZG[:, :n], in_=ZG[:, :n], func=AF.Silu)
            I(k, "dve", "tensor_tensor", [OS, ZG], [OS], out=OS[:, :n], in0=OS[:, :n], in1=ZG[:, :n], op=ALU.mult)
            k.dma("pool", yo[h, :, s0:s1], OS[:, :n], reads=[OS], is_output=True)
    k.finish()
    return k


RW_C = 64
import os
SUBDBG = int(os.environ.get('SUBDBG', '9'))


def build_RW(Tc, Tl, SEG, NP=2, dbg=9):
    k = KB()
    T = Tc + Tl
    C = RW_C
    NCH = SEG // C
    zr = [k.dram(f"zr{d}", [NP, 3, 128, T], kind="ExternalInput") for d in range(2)]
    zl = [k.dram(f"zl{d}", [128, T], kind="ExternalInput") for d in range(2)]
    zgd = k.dram("zgd", [2, 128, T], kind="ExternalInput")
    prm = k.dram("prm", [128, NP, 12], kind="ExternalInput")
    mul = k.dram("mul", [128, 4], kind="ExternalInput")
    wup = k.dram("wup", [128, 2, NP, 128], kind="ExternalInput")
    gup = k.dram("gup", [128, 2, NP, 128], kind="ExternalInput")
    idn2 = k.dram("idn2", [128, 256], kind="ExternalInput")
    msk = k.dram("msk", [128, 128], kind="ExternalInput")
    blk = k.dram("blk", [128, 128], kind="ExternalInput")
    yo = k.dram("yo", [NP, 128, T], kind="ExternalOutput")
    P = k.sbuf([128, NP, 12], name="P"); MUL = k.sbuf([128, 4], name="MUL")
    WUP = k.sbuf([128, 2, NP, 128], name="WUP"); GUP = k.sbuf([128, 2, NP, 128], name="GUP")
    IDN2 = k.sbuf([128, 256], name="IDN2"); MS = k.sbuf([128, 128], name="MS"); BLK = k.sbuf([128, 128], name="BLK")
    for i, (dst, src) in enumerate([(P, prm), (MUL, mul), (WUP, wup), (GUP, gup), (IDN2, idn2), (MS, msk), (BLK, blk)]):
        k.dma(["sp", "act", "pool"][i % 3], dst[:], src[:], writes=[dst])
    OMM = k.sbuf([128, NP, 3], name="OMM"); HMU = k.sbuf([128, NP, 3], name="HMU")
    OML = k.sbuf([128, 4], name="OML"); HML = k.sbuf([128, 4], name="HML"); OMKA = k.sbuf([128, NP, 1], name="OMKA")
    I(k, "dve", "tensor_scalar", [P], [OMM], out=OMM[:], in0=P[:, :, 9:12], scalar1=-1.0, scalar2=1.0, op0=ALU.mult, op1=ALU.add)
    I(k, "dve", "tensor_scalar", [P], [HMU], out=HMU[:], in0=P[:, :, 9:12], scalar1=0.5, scalar2=None, op0=ALU.mult)
    I(k, "dve", "tensor_scalar", [MUL], [OML], out=OML[:], in0=MUL[:], scalar1=-1.0, scalar2=1.0, op0=ALU.mult, op1=ALU.add)
    I(k, "dve", "tensor_scalar", [MUL], [HML], out=HML[:], in0=MUL[:], scalar1=0.5, scalar2=None, op0=ALU.mult)
    I(k, "dve", "tensor_scalar", [P], [OMKA], out=OMKA[:], in0=P[:, :, 5:6], scalar1=-1.0, scalar2=1.0, op0=ALU.mult, op1=ALU.add)
    ones = k.sbuf([128, SEG], name="ones")
    I(k, "dve", "memset", [], [ones], ones[:], 1.0)

    def sb(nm, shape=None):
        return k.sbuf(shape or [128, SEG], name=nm)
    ZH = sb("ZH", [128, SEG + 2]); TMPS = sb("TMPS")
    Rr = sb("Rr"); Kk = sb("Kk"); Vv = sb("Vv"); LR = sb("LR"); LW = sb("LW"); Aa = sb("Aa"); KKn = sb("KKn"); KS = sb("KS")
    CL = sb("CL"); EP = sb("EP"); EN = sb("EN"); T1 = sb("T1"); T2 = sb("T2")
    AR = k.sbuf([128, NCH, 2, C], name="AR")
    BDA = k.sbuf([128, NCH, 128], name="BDA"); BDB = k.sbuf([128, NCH, 128], name="BDB")
    BDK = k.sbuf([128, NCH, 128], name="BDK"); VBD = k.sbuf([128, NCH, 128], name="VBD")
    LC = k.sbuf([128, NCH], name="LC"); BASE = k.sbuf([128, NCH], name="BASE")
    for t_ in (BDA, BDB, BDK, VBD):
        I(k, "pool", "memset", [], [t_], t_[:], 0.0)
    YS = k.sbuf([128, T], name="YS"); BN = k.sbuf([128, T], name="BN")
    PN = [k.sbuf([128, 256], name=f"PN{i}") for i in range(2)]
    TT = k.sbuf([128, 256], name="TT")
    BDM = [k.sbuf([128, 128], name=f"BDM{i}") for i in range(2)]
    MRBK = [k.sbuf([128, 2, C], name=f"MRBK{i}") for i in range(2)]
    VMB = [k.sbuf([128, 128], name=f"VMB{i}") for i in range(2)]
    BKT = [k.sbuf([128, 256], name=f"BKT{i}") for i in range(2)]
    XS = k.sbuf([128, 128], name="XS"); US = [k.sbuf([128, 128], name=f"US{i}") for i in range(2)]
    SBD = [k.sbuf([128, 128], name=f"SBD{i}") for i in range(2)]
    for t_ in PN + BDM:
        I(k, "pool", "memset", [], [t_], t_[:], 0.0)
    banks = [k.psum([128, 512], name=f"rb{i}") for i in range(8)]

    def sub(b, a, c):
        return Tile(banks[b].ap[:, a:c], f"b{b}_{a}", bank=banks[b])
    PS1 = [sub(0, 0, 128), sub(0, 256, 384)]; PS2 = [sub(0, 128, 256), sub(0, 384, 512)]
    PSq = sub(1, 0, 256); PSt = sub(2, 0, 256)
    PSn = sub(3, 0, 128); PSv = sub(3, 128, 256); PSbk = sub(3, 256, 512)
    PSx = sub(4, 0, 128); PSu = sub(4, 128, 256); PSs = sub(4, 256, 384)
    PSy = [banks[5], banks[6]]
    PSb = banks[7]
    segs = seq_segments(Tc, Tl, SEG)

    def tshift(dst, src_ap_fn, seg, omm, hmu, rows=128):
        s0, s1, q0, q1 = seg
        n = s1 - s0
        lo, hi = max(s0 - 1, q0), min(s1 + 1, q1)
        I(k, "pool", "memset", [], [ZH], ZH[:, :n + 2], 0.0)
        k.dma("sp", ZH[:rows, lo - (s0 - 1):hi - (s0 - 1)], src_ap_fn(lo, hi), writes=[ZH])
        I(k, "dve", "tensor_tensor", [ZH], [TMPS], out=TMPS[:rows, :n], in0=ZH[:rows, 0:n], in1=ZH[:rows, 2:n + 2], op=ALU.add)
        I(k, "dve", "tensor_scalar", [TMPS, HMU, HML], [TMPS], out=TMPS[:rows, :n], in0=TMPS[:rows, :n], scalar1=hmu[:rows], scalar2=None, op0=ALU.mult)
        I(k, "dve", "scalar_tensor_tensor", [ZH, OMM, OML, TMPS], [dst], out=dst[:rows, :n], in0=ZH[:rows, 1:n + 1], scalar=omm[:rows],
          in1=TMPS[:rows, :n], op0=ALU.mult, op1=ALU.add)

    ci = 0
    yi = 0
    for p in range(NP):
        for d in range(2):
            S = SBD[0]
            I(k, "dve", "memset", [], [SBD[0]], SBD[0][:], 0.0)
            si = 0
            for seg in segs:
                s0, s1, q0, q1 = seg
                n = s1 - s0
                nch = n // C
                f0 = q0 + (q1 - s1); f1 = q0 + (q1 - s0)
                tshift(Rr, lambda lo, hi: zr[d][p, 0, :, lo:hi], seg, OMM[:, p, 0:1], HMU[:, p, 0:1])
                tshift(Kk, lambda lo, hi: zr[d][p, 1, :, lo:hi], seg, OMM[:, p, 1:2], HMU[:, p, 1:2])
                tshift(Vv, lambda lo, hi: zr[d][p, 2, :, lo:hi], seg, OMM[:, p, 2:3], HMU[:, p, 2:3])
                tshift(LR, lambda lo, hi: zl[d][:, lo:hi], seg, OML[:, d:d + 1], HML[:, d:d + 1])
                I(k, "act", "activation", [LR], [LR], out=LR[0:64, :n], in_=LR[0:64, :n], func=AF.Tanh)
                for c0 in range(0, n, 512):
                    cn = min(512, n - c0)
                    I(k, "pe", "matmul", [WUP, LR], [PSb], PSb[:, :cn], lhsT=WUP[0:64, d, p, :], rhs=LR[0:64, c0:c0 + cn], start=True, stop=True)
                    I(k, "act", "activation", [PSb, P], [LW], out=LW[:, c0:c0 + cn], in_=PSb[:, :cn], func=AF.Sigmoid, bias=P[:, p, d:d + 1], scale=1.0)
                    I(k, "pe", "matmul", [WUP, LR], [PSb], PSb[:, :cn], lhsT=WUP[64:128, d, p, :], rhs=LR[64:128, c0:c0 + cn], start=True, stop=True)
                    I(k, "act", "activation", [PSb, P], [Aa], out=Aa[:, c0:c0 + cn], in_=PSb[:, :cn], func=AF.Sigmoid, bias=P[:, p, 2 + d:3 + d], scale=1.0)
                I(k, "dve", "tensor_scalar", [LW], [LW], out=LW[:, :n], in0=LW[:, :n], scalar1=-float(np.exp(-0.5)), scalar2=None, op0=ALU.mult)
                I(k, "dve", "tensor_scalar", [Kk, P], [KKn], out=KKn[:, :n], in0=Kk[:, :n], scalar1=P[:, p, 4:5], scalar2=None, op0=ALU.mult)
                I(k, "act", "activation", [KKn], [T1], out=T1[:, :n], in_=KKn[:, :n], func=AF.Square)
                for c0 in range(0, n, 512):
                    cn = min(512, n - c0)
                    I(k, "pe", "matmul", [BLK, T1], [PSb], PSb[:, :cn], lhsT=BLK[:], rhs=T1[:, c0:c0 + cn], start=True, stop=True)
                    I(k, "dve", "tensor_scalar", [PSb], [T2], out=T2[:, c0:c0 + cn], in0=PSb[:, :cn], scalar1=1e-12, scalar2=None, op0=ALU.add)
                I(k, "act", "activation", [T2], [T2], out=T2[:, :n], in_=T2[:, :n], func=AF.Sqrt)
                I(k, "dve", "reciprocal", [T2], [T2], out=T2[:, :n], in_=T2[:, :n])
                I(k, "dve", "tensor_tensor", [KKn, T2], [KKn], out=KKn[:, :n], in0=KKn[:, :n], in1=T2[:, :n], op=ALU.mult)
                I(k, "dve", "tensor_scalar", [Aa, P, OMKA], [KS], out=KS[:, :n], in0=Aa[:, :n], scalar1=P[:, p, 5:6], scalar2=OMKA[:, p, 0:1],
                  op0=ALU.mult, op1=ALU.add)
                I(k, "dve", "tensor_tensor", [KS, Kk], [KS], out=KS[:, :n], in0=KS[:, :n], in1=Kk[:, :n], op=ALU.mult)
                I(k, "dve", "scalar_tensor_tensor", [Rr, P, KS], [T1], out=T1[:, :n], in0=Rr[:, :n], scalar=P[:, p, 6:7], in1=KS[:, :n],
                  op0=ALU.mult, op1=ALU.mult)
                for c0 in range(0, n, 512):
                    cn = min(512, n - c0)
                    I(k, "pe", "matmul", [BLK, T1], [PSb], PSb[:, :cn], lhsT=BLK[:], rhs=T1[:, c0:c0 + cn], start=True, stop=True)
                    I(k, "dve", "tensor_tensor", [PSb, Vv], [T2], out=T2[:, c0:c0 + cn], in0=PSb[:, :cn], in1=Vv[:, c0:c0 + cn], op=ALU.mult)
                if d == 0:
                    I(k, "dve", "tensor_copy", [T2], [BN], out=BN[:, s0:s1], in_=T2[:, :n])
                else:
                    I(k, "dve", "tensor_tensor", [T2, BN], [BN], out=BN[:, f0:f1], in0=BN[:, f0:f1], in1=T2[:, :n][:, ::-1], op=ALU.add)
                I(k, "dve", "tensor_tensor_scan", [ones, LW], [CL], out=CL[:, :n], data0=ones[:, :n], data1=LW[:, :n], initial=0.0,
                  op0=ALU.mult, op1=ALU.add)
                C3 = CL[:, :n].rearrange("p (c t) -> p c t", t=C)
                I(k, "dve", "memset", [], [BASE], BASE[:, 0:1], 0.0)
                if nch > 1:
                    I(k, "dve", "tensor_copy", [CL], [BASE], out=BASE[:, 1:nch], in_=C3[:, 0:nch - 1, C - 1])
                I(k, "dve", "tensor_tensor", [CL, BASE], [CL], out=C3, in0=C3, in1=BASE[:, :nch].unsqueeze(2).to_broadcast([128, nch, C]), op=ALU.subtract)
                I(k, "act", "activation", [CL], [EP], out=EP[:, :n], in_=CL[:, :n], func=AF.Exp)
                I(k, "act", "activation", [CL], [EN], out=EN[:, :n], in_=CL[:, :n], func=AF.Exp, scale=-1.0)
                I(k, "dve", "tensor_copy", [EP], [LC], out=LC[:, :nch], in_=EP[:, :n].rearrange("p (c t) -> p c t", t=C)[:, :, C - 1])
                AR4 = AR[:, :nch]
                v3 = lambda tl: tl[:, :n].rearrange("p (c t) -> p c t", t=C)
                I(k, "dve", "tensor_tensor", [Rr, EP], [AR], out=AR4[:, :, 1, :], in0=v3(Rr), in1=v3(EP), op=ALU.mult)
                I(k, "dve", "tensor_tensor", [CL, LW], [T1], out=T1[:, :n], in0=CL[:, :n], in1=LW[:, :n], op=ALU.subtract)
                I(k, "act", "activation", [T1], [T1], out=T1[:, :n], in_=T1[:, :n], func=AF.Exp)
                I(k, "dve", "scalar_tensor_tensor", [KKn, T1], [AR], out=AR4[:, :, 0, :], in0=v3(KKn), scalar=-1.0, in1=v3(T1), op0=ALU.mult, op1=ALU.mult)
                I(k, "dve", "tensor_tensor", [KKn, Aa], [T1], out=T1[:, :n], in0=KKn[:, :n], in1=Aa[:, :n], op=ALU.mult)
                I(k, "dve", "tensor_tensor", [T1, EN], [T1], out=T1[:, :n], in0=T1[:, :n], in1=EN[:, :n], op=ALU.mult)
                I(k, "dve", "tensor_tensor", [KS, EN], [T2], out=T2[:, :n], in0=KS[:, :n], in1=EN[:, :n], op=ALU.mult)
                for (bd, src, isar) in ((BDA, AR, True), (BDB, T1, False), (BDK, T2, False), (VBD, Vv, False)):
                    for hh in range(2):
                        pr = slice(hh * 64, hh * 64 + 64)
                        s_ap = AR4[pr, :, 0, :] if isar else v3(src)[pr]
                        I(k, "act" if hh else "dve", "activation" if hh else "tensor_copy", [src], [bd],
                          **({"out": bd[pr, :nch, hh * 64:hh * 64 + 64], "in_": s_ap, "func": AF.Copy} if hh else
                             {"out": bd[pr, :nch, hh * 64:hh * 64 + 64], "in_": s_ap}))
                for c in range(nch if dbg >= 2 else 0):
                    a = ci % 2
                    ci += 1
                    ARc = AR[:, c].rearrange("p a t -> p (a t)")
                    I(k, "pe", "matmul", [BDB, AR], [PS1[a]], PS1[a][:, :], lhsT=BDB[:, c, :], rhs=ARc, start=True, stop=True)
                    if dbg == 2 and SUBDBG >= 1:
                        I(k, "pe", "matmul", [BDK, AR], [PS2[a]], PS2[a][:, :], lhsT=BDK[:, c, :], rhs=ARc, start=True, stop=True)
                    if dbg == 2 and SUBDBG < 2:
                        continue
                    if dbg > 2:
                        I(k, "pe", "matmul", [BDK, AR], [PS2[a]], PS2[a][:, :], lhsT=BDK[:, c, :], rhs=ARc, start=True, stop=True)
                    pn = PN[0]
                    if dbg == 2 and ci > int(os.environ.get('LIMC', '99999')):
                        continue
                    for hh in (range(2) if dbg > 2 else {'1': [0], '2': [0, 1], '3': [1], '4': [1, 0]}[os.environ.get('LIMH', '2')]):
                        pr = slice(hh * 64, hh * 64 + 64)
                        if os.environ.get('NOMS'):
                            I(k, "dve", "tensor_copy", [PS1[a], PS2[a]], [pn], out=pn[pr, hh * 64:hh * 64 + 64], in_=PS1[a][pr, 0:64])
                        else:
                            I(k, "dve", "tensor_tensor", [PS1[a], PS2[a], MS], [pn], out=pn[pr, hh * 64:hh * 64 + 64], in0=PS1[a][pr, 0:64], in1=MS[pr, 0:64], op=ALU.mult)
                        if dbg == 2 and SUBDBG < 3:
                            continue
                        I(k, "dve", "tensor_tensor", [PS2[a], MS], [BDM[a]], out=BDM[a][pr, hh * 64:hh * 64 + 64], in0=PS2[a][pr, 0:64], in1=MS[pr, 0:64], op=ALU.mult)
                    if dbg == 2 and SUBDBG < 4:
                        continue
                    I(k, "dve", "tensor_tensor", [PS1[a], MS], [MRBK[a]], out=MRBK[a][:, 0, :], in0=PS1[a][:, 64:128], in1=MS[:, 64:128], op=ALU.mult)
                    I(k, "dve", "tensor_tensor", [PS2[a], MS], [MRBK[a]], out=MRBK[a][:, 1, :], in0=PS2[a][:, 64:128], in1=MS[:, 64:128], op=ALU.mult)
                    if dbg < 3:
                        continue
                    I(k, "pe", "transpose", [pn, IDN2], [PSn], PSn[:, :], pn[:, 0:128], IDN2[:, 0:128])
                    I(k, "act", "activation", [PSn], [pn], out=pn[:, 128:256], in_=PSn[:, :], func=AF.Copy)
                    I(k, "dve", "tensor_tensor", [pn, IDN2], [TT], out=TT[:], in0=pn[:], in1=IDN2[:], op=ALU.add)
                    cur = 0
                    for lvl in range(1, 6):
                        last = (lvl == 5)
                        pc = PN[cur]; pnx = PN[1 - cur]
                        I(k, "pe", "matmul", [pc], [PSq], PSq[:, 0:128], lhsT=pc[:, 128:256], rhs=pc[:, 0:128], start=True, stop=True)
                        if not last:
                            I(k, "pe", "matmul", [pc], [PSq], PSq[:, 128:256], lhsT=pc[:, 0:128], rhs=pc[:, 128:256], start=True, stop=True)
                        w_ = 128 if last else 256
                        I(k, "act", "activation", [PSq], [pnx], out=pnx[:, 0:w_], in_=PSq[:, 0:w_], func=AF.Copy)
                        I(k, "pe", "matmul", [TT, pnx], [PSt], PSt[:, 0:128], lhsT=TT[:, 128:256], rhs=pnx[:, 0:128], start=True, stop=True)
                        if not last:
                            I(k, "pe", "matmul", [TT, pnx], [PSt], PSt[:, 128:256], lhsT=pnx[:, 0:128], rhs=TT[:, 128:256], start=True, stop=True)
                        I(k, "dve", "tensor_tensor", [PSt, TT], [TT], out=TT[:, 0:w_], in0=PSt[:, 0:w_], in1=TT[:, 0:w_], op=ALU.add)
                        cur = 1 - cur
                    if dbg < 4:
                        continue
                    I(k, "pe", "transpose", [VBD, IDN2], [PSv], PSv[:, :], VBD[:, c, :], IDN2[:, 0:128])
                    I(k, "act", "activation", [PSv], [VMB[a]], out=VMB[a][:], in_=PSv[:, :], func=AF.Copy)
                    I(k, "pe", "transpose", [BDB, IDN2], [PSbk], PSbk[:, 0:128], BDB[:, c, :], IDN2[:, 0:128])
                    I(k, "pe", "transpose", [BDK, IDN2], [PSbk], PSbk[:, 128:256], BDK[:, c, :], IDN2[:, 0:128])
                    I(k, "act", "activation", [PSbk], [BKT[a]], out=BKT[a][:], in_=PSbk[:, :], func=AF.Copy)
                    if dbg < 5:
                        continue
                    Sc = SBD[si % 2]; Sn = SBD[(si + 1) % 2]; si += 1
                    I(k, "pe", "matmul", [BDA, Sc], [PSx], PSx[:, :], lhsT=BDA[:, c, :], rhs=Sc[:], start=True, stop=False)
                    I(k, "pe", "matmul", [BDM[a], VMB[a]], [PSx], PSx[:, :], lhsT=BDM[a][:], rhs=VMB[a][:], start=False, stop=True)
                    I(k, "act", "activation", [PSx], [XS], out=XS[:], in_=PSx[:, :], func=AF.Copy)
                    I(k, "pe", "matmul", [TT, XS], [PSu], PSu[:, :], lhsT=TT[:, 0:128], rhs=XS[:], start=True, stop=True)
                    I(k, "act", "activation", [PSu], [US[a]], out=US[a][:], in_=PSu[:, :], func=AF.Copy)
                    if c % 8 == 0:
                        py = PSy[yi % 2]; yi += 1
                    oc = (c % 8) * C
                    I(k, "pe", "matmul", [Sc, AR], [py], py[:, oc:oc + C], lhsT=Sc[:], rhs=AR[:, c, 1, :], start=True, stop=False)
                    I(k, "pe", "matmul", [US[a], MRBK[a]], [py], py[:, oc:oc + C], lhsT=US[a][:], rhs=MRBK[a][:, 0, :], start=False, stop=False)
                    I(k, "pe", "matmul", [VMB[a], MRBK[a]], [py], py[:, oc:oc + C], lhsT=VMB[a][:], rhs=MRBK[a][:, 1, :], start=False, stop=True)
                    I(k, "pe", "matmul", [BKT[a], US[a]], [PSs], PSs[:, :], lhsT=BKT[a][:, 0:128], rhs=US[a][:], start=True, stop=False)
                    I(k, "pe", "matmul", [BKT[a], VMB[a]], [PSs], PSs[:, :], lhsT=BKT[a][:, 128:256], rhs=VMB[a][:], start=False, stop=True)
                    I(k, "dve", "tensor_tensor", [PSs, Sc], [Sn], out=Sn[:], in0=PSs[:, :], in1=Sc[:], op=ALU.add)
                    I(k, "dve", "tensor_scalar", [Sn, LC], [Sn], out=Sn[:], in0=Sn[:], scalar1=LC[:, c:c + 1], scalar2=None, op0=ALU.mult)
                    if c % 8 == 7 or c == nch - 1:
                        c0 = (c // 8) * 8 * C
                        w = (c % 8 + 1) * C
                        if d == 0:
                            I(k, "act", "activation", [py], [YS], out=YS[:, s0 + c0:s0 + c0 + w], in_=py[:, 0:w], func=AF.Copy)
                        else:
                            I(k, "dve", "tensor_tensor", [py, YS], [YS], out=YS[:, f1 - c0 - w:f1 - c0], in0=YS[:, f1 - c0 - w:f1 - c0],
                              in1=py[:, 0:w][:, ::-1], op=ALU.add)
                if si % 2 == 1:
                    pass
        G0 = Rr; G1 = Kk; GG = Vv; YC = KS; SQ = T1; RS = T2; MN = CL
        for seg in (segs if dbg >= 6 else []):
            s0, s1, q0, q1 = seg
            n = s1 - s0
            tshift(G0, lambda lo, hi: zgd[0, :, lo:hi], seg, OML[:, 2:3], HML[:, 2:3])
            tshift(G1, lambda lo, hi: zgd[1, 0:32, lo:hi], seg, OML[:, 3:4], HML[:, 3:4], rows=32)
            I(k, "act", "activation", [G0], [G0], out=G0[:, :n], in_=G0[:, :n], func=AF.Sigmoid)
            I(k, "act", "activation", [G1], [G1], out=G1[0:32, :n], in_=G1[0:32, :n], func=AF.Sigmoid)
            for c0 in range(0, n, 512):
                cn = min(512, n - c0)
                I(k, "pe", "matmul", [GUP, G0], [PSb], PSb[:, :cn], lhsT=GUP[:, 0, p, :], rhs=G0[:, c0:c0 + cn], start=True, stop=False)
                I(k, "pe", "matmul", [GUP, G1], [PSb], PSb[:, :cn], lhsT=GUP[0:32, 1, p, :], rhs=G1[0:32, c0:c0 + cn], start=False, stop=True)
                I(k, "act", "activation", [PSb], [GG], out=GG[:, c0:c0 + cn], in_=PSb[:, :cn], func=AF.Copy)
                I(k, "pe", "matmul", [BLK, YS], [PSb], PSb[:, :cn], lhsT=BLK[:], rhs=YS[:, s0 + c0:s0 + c0 + cn], start=True, stop=True)
                I(k, "dve", "scalar_tensor_tensor", [PSb, YS], [YC], out=YC[:, c0:c0 + cn], in0=PSb[:, :cn], scalar=-1.0 / 64, in1=YS[:, s0 + c0:s0 + c0 + cn],
                  op0=ALU.mult, op1=ALU.add)
                I(k, "act", "activation", [YC], [SQ], out=SQ[:, c0:c0 + cn], in_=YC[:, c0:c0 + cn], func=AF.Square)
                I(k, "pe", "matmul", [BLK, SQ], [PSb], PSb[:, :cn], lhsT=BLK[:], rhs=SQ[:, c0:c0 + cn], start=True, stop=True)
                I(k, "dve", "tensor_scalar", [PSb], [RS], out=RS[:, c0:c0 + cn], in0=PSb[:, :cn], scalar1=1.0 / 64, scalar2=64e-5, op0=ALU.mult, op1=ALU.add)
            I(k, "act", "activation", [RS], [RS], out=RS[:, :n], in_=RS[:, :n], func=AF.Sqrt)
            I(k, "dve", "reciprocal", [RS], [RS], out=RS[:, :n], in_=RS[:, :n])
            I(k, "dve", "tensor_tensor", [YC, RS], [YC], out=YC[:, :n], in0=YC[:, :n], in1=RS[:, :n], op=ALU.mult)
            I(k, "dve", "tensor_scalar", [YC, P], [YC], out=YC[:, :n], in0=YC[:, :n], scalar1=P[:, p, 7:8], scalar2=P[:, p, 8:9], op0=ALU.mult, op1=ALU.add)
            I(k, "dve", "tensor_tensor", [YC, BN], [YC], out=YC[:, :n], in0=YC[:, :n], in1=BN[:, s0:s1], op=ALU.add)
            I(k, "dve", "tensor_tensor", [YC, GG], [YC], out=YC[:, :n], in0=YC[:, :n], in1=GG[:, :n], op=ALU.mult)
            k.dma("pool", yo[p, :, s0:s1], YC[:, :n], reads=[YC], is_output=True)
    k.finish()
    return k


B_, S_, D_, L_, TC_ = 2, 8192, 4096, 4, 256
GW_ = 1024
T_ = TC_ + S_
OFF_HG, OFF_RW, OFF_LRU, OFF_POOL = 0, 5120, 8608, 10656
_PROG = {}
N_LAUNCH = [0]


import time as _time
_VERB = bool(os.environ.get("KVERB"))


def _prog(name, fn):
    if name not in _PROG:
        t = _time.time()
        _PROG[name] = fn()
        if _VERB:
            print(f"[build {name}] {_time.time() - t:.1f}s instr={_PROG[name].n_instr}", flush=True)
    return _PROG[name]


def _run(k, in_maps):
    N_LAUNCH[0] += 1
    t = _time.time()
    res = run_bass_kernel_spmd(k.nc, in_maps, core_ids=list(range(len(in_maps))))
    if _VERB:
        nb = sum(v.nbytes for m in in_maps for v in m.values())
        print(f"[run] {_time.time() - t:.1f}s in_bytes={nb / 1e6:.0f}MB", flush=True)
    return res.results


def _c(a):
    return np.ascontiguousarray(a, dtype=np.float32)


def rw_consts():
    idn2 = np.concatenate([np.eye(128), np.eye(128)], 1).astype(np.float32)
    msk = np.zeros((128, 128), np.float32)
    for p in range(128):
        i = p % 64
        msk[p, 0:64] = (np.arange(64) > i)
        msk[p, 64:128] = (np.arange(64) >= i)
    blk = np.zeros((128, 128), np.float32)
    blk[:64, :64] = 1
    blk[64:, 64:] = 1
    return {"idn2": idn2, "msk": msk, "blk": blk}


def rw_inputs(z, zf, j, prm):
    mu, w0, w_up, a0, a_up, g_up, k_k, k_a, r_k, ln_w, ln_b = prm
    zz = [z, zf]
    T = z.shape[0]
    ins = {}
    for d in range(2):
        zr = np.zeros((2, 3, 128, T), np.float32)
        for p in range(2):
            for i in range(3):
                c0 = i * 1024 + j * 256 + p * 128
                zr[p, i] = zz[d][:, c0:c0 + 128].T
        ins[f"zr{d}"] = zr
        ins[f"zl{d}"] = _c(np.concatenate([zz[d][:, 3072 + 64 * d:3136 + 64 * d], zz[d][:, 3200 + 64 * d:3264 + 64 * d]], 1).T)
    zgd = np.zeros((2, 128, T), np.float32)
    zgd[0] = z[:, 3328:3456].T
    zgd[1, :32] = z[:, 3456:3488].T
    ins["zgd"] = zgd
    P = np.zeros((128, 2, 12), np.float32)
    for p in range(2):
        ch = slice(j * 256 + p * 128, j * 256 + p * 128 + 128)
        cols = [w0[0][ch], w0[1][ch], a0[0][ch], a0[1][ch], k_k[ch], k_a[ch], r_k[ch], ln_w[ch], ln_b[ch],
                mu[0:1024][ch], mu[1024:2048][ch], mu[2048:3072][ch]]
        P[:, p, :] = np.stack(cols, 1)
    ins["prm"] = P
    mul = np.zeros((128, 4), np.float32)
    for d in range(2):
        mul[:, d] = np.concatenate([mu[3072 + 64 * d:3136 + 64 * d], mu[3200 + 64 * d:3264 + 64 * d]])
    mul[:, 2] = mu[3328:3456]
    mul[:32, 3] = mu[3456:3488]
    ins["mul"] = mul
    wup = np.zeros((128, 2, 2, 128), np.float32)
    gup = np.zeros((128, 2, 2, 128), np.float32)
    for p in range(2):
        ch = slice(j * 256 + p * 128, j * 256 + p * 128 + 128)
        for d in range(2):
            wup[:64, d, p] = w_up[d][:, ch]
            wup[64:, d, p] = a_up[d][:, ch]
        gup[:, 0, p] = g_up[0:128, ch]
        gup[:32, 1, p] = g_up[128:160, ch]
    ins["wup"] = wup
    ins["gup"] = gup
    ins.update(rw_consts())
    return ins


def hg_inputs(z, zf, j, layer, hg_lb, gnw):
    gw = GW_
    L = hg_lb.shape[1]

    def part(zz, idx):
        return zz[:, idx * gw + j * 256: idx * gw + (j + 1) * 256].T.reshape(2, 128, -1)
    zd0 = np.stack([part(z, 0), part(z, 1), part(z, 3)], 1)
    zd1 = np.stack([part(zf, 0), part(zf, 2), part(zf, 3)], 1)
    zg = part(z, 4)
    lbp = _c(hg_lb[:, :, j * 256:(j + 1) * 256].reshape(2, L, 2, 128).transpose(3, 2, 0, 1))
    lmask = np.zeros((128, L), np.float32)
    lmask[:, 1:layer + 1] = 1
    msk = np.triu(np.ones((HG_C, HG_C), np.float32))
    return {"zd0": _c(zd0), "zd1": _c(zd1), "zg": _c(zg), "lbp": lbp, "lmask": lmask,
            "gn": _c(gnw.reshape(128, 1)), "msk": msk, "idn": np.eye(128, dtype=np.float32)}


def lru_inputs(z, j, conv_w, conv_b, wa, ba, wx, bx, lam):
    gw = GW_
    ch = slice(j * 256, (j + 1) * 256)
    zx = _c(z[:, :gw][:, ch].T.reshape(2, 128, -1))
    zy = _c(z[:, gw:][:, ch].T.reshape(2, 128, -1))
    cols = np.stack([conv_w[0], conv_w[1], conv_w[2], conv_w[3], conv_b, ba[0], ba[1], bx[0], bx[1], lam[0], lam[1]], -1)
    prm = _c(cols[ch].reshape(2, 128, 11).transpose(1, 0, 2))
    WA = _c(wa[:, 2 * j:2 * j + 2].transpose(2, 1, 0, 3))
    WX = _c(wx[:, 2 * j:2 * j + 2].transpose(2, 1, 0, 3))
    return {"zx": zx, "zy": zy, "prm": prm, "wa": WA, "wx": WX}


def _pad_lines(a):
    ch, nl, lw = a.shape
    o = np.zeros((ch, nl, lw + 16), np.float32)
    o[:, :, 8:8 + lw] = a
    return o.reshape(8, 128, nl, lw + 16)


def pool_inputs(zl, zc, j, layer, lin_w, scale):
    S_ = zl.shape[0]
    TC_ = zc.shape[0]
    rows = S_ // 64
    g3 = zl.reshape(rows, 64, GW_)
    if layer % 2 == 0:
        nl = rows // 4
        lines = g3[j * nl:(j + 1) * nl].transpose(2, 0, 1)
        n_line = 64
    else:
        nl = 64 // 4
        lines = g3[:, j * nl:(j + 1) * nl].transpose(2, 1, 0)
        n_line = rows
    cpad = np.zeros((TC_ + 16, GW_), np.float32)
    cpad[8:8 + TC_] = zc
    seg = TC_ // 4
    zp1 = _c(cpad[j * seg:j * seg + seg + 16].T.reshape(8, 128, 1, seg + 16))
    inv0 = np.tile(pool_invcount(n_line, np.arange(n_line))[None], (128, 1, 1))
    inv1 = np.tile(pool_invcount(TC_, np.arange(j * seg, (j + 1) * seg))[None], (128, 1, 1))
    LIN = _c(lin_w.reshape(4, 2, 128, 256).transpose(2, 0, 1, 3))
    SCL = _c(scale.reshape(8, 128).T)
    return {"zp0": _c(_pad_lines(lines)), "zp1": zp1, "inv0": _c(inv0), "inv1": _c(inv1), "lin": LIN, "scl": SCL}


def kernel(x, c, ctx, c_ctx, w_ada, b_ada, norm_g, w_in, w_out, hg_lb, hg_gnorm, rw_mu, rw_w0, rw_w_up, rw_a0, rw_a_up,
           rw_g_up, rw_kk, rw_ka, rw_rk, rw_ln_w, rw_ln_b, lru_conv_w, lru_conv_b, lru_wa, lru_ba, lru_wx, lru_bx,
           lru_lam, pool_w, pool_scale, moe_wc, moe_bc, moe_wf, moe_bf, moe_w_gu, moe_w_down, final_g):
    A = lambda a: np.asarray(a, dtype=np.float32)
    x, c, ctx, c_ctx = A(x), A(c), A(ctx), A(c_ctx)
    NC = 8
    B_, S_, D_ = x.shape
    TC_ = ctx.shape[1]
    L_ = w_in.shape[0]
    T_ = TC_ + S_
    cfg = Cfg(ntiles_lat=S_ // 4 // 512, nctx=TC_ // 4)
    NCOL = 6 * D_ // NC
    k0 = _prog(f"P0_{L_}", lambda: build_P0(D_, NCOL // 128, L_))
    crow = np.stack([c[0], c[1], c_ctx])
    cT = _c(crow.T.reshape(D_ // 128, 128, 3).transpose(1, 0, 2))
    maps = []
    for i in range(NC):
        wl = np.stack([wlayout(A(w_ada[l])[:, i * NCOL:(i + 1) * NCOL]) for l in range(L_)])
        bl = _c(A(b_ada)[:, i * NCOL:(i + 1) * NCOL].reshape(L_, NCOL // 128, 128).transpose(2, 0, 1))
        maps.append({"w": wl, "cT": cT, "b": bl})
    res = _run(k0, maps)
    mod = np.zeros((L_, 3, 6 * D_), np.float32)
    for i in range(NC):
        o = res[i]["out"]
        mod[:, :, i * NCOL:(i + 1) * NCOL] = o.transpose(1, 3, 2, 0).reshape(L_, 3, NCOL)
    del maps, res
    mod = mod.reshape(L_, 3, 6, D_)

    xl = x.copy()
    xc = ctx.copy()
    nlat = S_ // 4
    nctx = TC_ // 4

    def tok_slice(arr_l, arr_c, b, j):
        return np.concatenate([arr_l[b, j * nlat:(j + 1) * nlat], arr_c[b, j * nctx:(j + 1) * nctx]], 0)

    sel = np.zeros((16, 16, 128), np.float32)
    for e in range(16):
        sel[e, e, :] = 1
    idn = np.eye(128, dtype=np.float32)

    for l in range(L_):
        kA = _prog(f"A{S_}", lambda: build_A(cfg))
        wl = wlayout(A(w_in[l]))
        maps = []
        for b in range(B_):
            for j in range(4):
                prm = np.stack([np.stack([mod[l, b, 0], mod[l, b, 1], A(norm_g)[l, 0]]),
                                np.stack([mod[l, 2, 0], mod[l, 2, 1], A(norm_g)[l, 0]])])
                maps.append({"xT": fm(tok_slice(xl, xc, b, j)), "w": wl, "prm": vec_fm(prm)})
        res = _run(kA, maps)
        W = cfg.NCC_IN * 128
        zl = np.zeros((B_, S_, W), np.float32)
        zc = np.zeros((B_, TC_, W), np.float32)
        for b in range(B_):
            for j in range(4):
                zt = res[b * 4 + j]["zT"].reshape(W, cfg.NTOK).T
                zl[b, j * nlat:(j + 1) * nlat] = zt[:nlat]
                zc[b, j * nctx:(j + 1) * nctx] = zt[nlat:]
        del maps, res, wl
        y_l = np.zeros((B_, S_, 4 * GW_), np.float32)
        y_c = np.zeros((B_, TC_, 4 * GW_), np.float32)
        zcat = [np.concatenate([zc[b], zl[b]], 0) for b in range(B_)]
        zflip = [np.concatenate([zc[b][::-1], zl[b][::-1]], 0) for b in range(B_)]

        def put(mix, b, j, yT):
            c0 = mix * GW_ + j * 256
            y_c[b][:, c0:c0 + 256] = yT[:, :TC_].T
            y_l[b][:, c0:c0 + 256] = yT[:, TC_:].T

        kH = _prog(f"HG{S_}", lambda: build_HG(TC_, S_, 1024, L=L_))
        maps = [hg_inputs(zcat[b][:, OFF_HG:OFF_RW], zflip[b][:, OFF_HG:OFF_RW], j, l, A(hg_lb), A(hg_gnorm)[l])
                for b in range(B_) for j in range(4)]
        res = _run(kH, maps)
        for b in range(B_):
            for j in range(4):
                put(0, b, j, res[b * 4 + j]["yo"].reshape(256, T_))
        kR = _prog(f"RW{S_}", lambda: build_RW(TC_, S_, 512))
        rprm = (A(rw_mu)[l], A(rw_w0)[l], A(rw_w_up)[l], A(rw_a0)[l], A(rw_a_up)[l], A(rw_g_up)[l], A(rw_kk)[l],
                A(rw_ka)[l], A(rw_rk)[l], A(rw_ln_w)[l], A(rw_ln_b)[l])
        maps = [rw_inputs(zcat[b][:, OFF_RW:OFF_LRU], zflip[b][:, OFF_RW:OFF_LRU], j, rprm) for b in range(B_) for j in range(4)]
        res = _run(kR, maps)
        for b in range(B_):
            for j in range(4):
                put(1, b, j, res[b * 4 + j]["yo"].reshape(256, T_))
        kL = _prog(f"LRU{S_}", lambda: build_LRU(TC_, S_, 2048))
        maps = [lru_inputs(zcat[b][:, OFF_LRU:OFF_POOL], j, A(lru_conv_w)[l], A(lru_conv_b)[l], A(lru_wa)[l], A(lru_ba)[l],
                           A(lru_wx)[l], A(lru_bx)[l], A(lru_lam)[l]) for b in range(B_) for j in range(4)]
        res = _run(kL, maps)
        for b in range(B_):
            for j in range(4):
                put(2, b, j, res[b * 4 + j]["yo"].reshape(256, T_))
        rows = S_ // 64
        grp = [(rows // 4, 64), (1, TC_ // 4)] if l % 2 == 0 else [(16, rows), (1, TC_ // 4)]
        kP = _prog(f"POOL{l % 2}_{S_}", lambda: build_POOL(grp))
        maps = [pool_inputs(zl[b][:, OFF_POOL:OFF_POOL + GW_], zc[b][:, OFF_POOL:OFF_POOL + GW_], j, l, A(pool_w)[l], A(pool_scale)[l])
                for b in range(B_) for j in range(4)]
        res = _run(kP, maps)
        for b in range(B_):
            yg = y_l[b].reshape(rows, 64, 4 * GW_)
            for j in range(4):
                r0 = res[b * 4 + j]["yp0"]
                if l % 2 == 0:
                    nl = rows // 4
                    yg[j * nl:(j + 1) * nl, :, 3 * GW_:] = r0.reshape(GW_, nl, 64).transpose(1, 2, 0)
                else:
                    nl = 16
                    yg[:, j * nl:(j + 1) * nl, 3 * GW_:] = r0.reshape(GW_, nl, rows).transpose(2, 1, 0)
                y_c[b][j * nctx:(j + 1) * nctx, 3 * GW_:] = res[b * 4 + j]["yp1"].reshape(GW_, nctx).T
        del maps, res, zcat, zflip, zl, zc
        final = (l == L_ - 1)
        kC = _prog(("C1" if final else "C0") + str(S_), lambda: build_C(cfg, final=final))
        wo = wlayout(A(w_out[l]))
        wgu = A(moe_w_gu[l])
        wgu_l = np.stack([wlayout(wgu[e]) for e in range(16)])
        wdn = wlayout(A(moe_w_down[l]).reshape(16 * 256, D_))
        wcf = np.concatenate([A(moe_wc[l]), A(moe_wf[l])], 1)
        wcf = _c(wcf.reshape(D_ // 128, 128, 20).transpose(1, 0, 2))
        bcf = _c(np.tile(np.concatenate([A(moe_bc[l]), A(moe_bf[l])])[None], (128, 1)))
        maps = []
        for b in range(B_):
            for j in range(4):
                g1 = A(norm_g)[l, 1]
                fg = A(final_g)
                prm = np.stack([np.stack([mod[l, b, 2], mod[l, b, 3], mod[l, b, 4], mod[l, b, 5], g1, fg]),
                                np.stack([mod[l, 2, 2], mod[l, 2, 3], mod[l, 2, 4], mod[l, 2, 5], g1, fg])])
                maps.append({"xT": fm(tok_slice(xl, xc, b, j)), "yT": fm(tok_slice(y_l, y_c, b, j)), "w_out": wo,
                             "prm": vec_fm(prm), "wcf": wcf, "bcf": bcf, "w_gu": wgu_l, "w_dn": wdn, "sel": sel, "idn": idn})
        res = _run(kC, maps)
        for b in range(B_):
            for j in range(4):
                xt = unfm(res[b * 4 + j]["xo"])
                xl[b, j * nlat:(j + 1) * nlat] = xt[:nlat]
                xc[b, j * nctx:(j + 1) * nctx] = xt[nlat:]
        del maps, res, wo, wgu_l, wdn, y_l, y_c
    return xl
```

```python
import os
import numpy as np
import concourse.bass as bass
import concourse.mybir as mybir
from concourse.bass_utils import run_bass_kernel_spmd

F32 = mybir.dt.float32
BF16 = mybir.dt.bfloat16
AF = mybir.ActivationFunctionType
ALU = mybir.AluOpType
AX = mybir.AxisListType


class Tile:
    __slots__ = ("ap", "last_w", "reads", "name", "bank", "pe_last")

    def __init__(self, ap, name="", bank=None):
        self.bank = bank
        self.pe_last = None
        self.ap = ap
        self.last_w = None
        self.reads = {}
        self.name = name

    def __getitem__(self, idx):
        return self.ap[idx]


class KB:
    def __init__(self, same_engine_sync=True, n_dma_sems=12):
        self.nc = bass.Bass("TRN2", target_bir_lowering=False)
        nc = self.nc
        self.eng = {"pe": nc.tensor, "act": nc.scalar, "dve": nc.vector, "pool": nc.gpsimd, "sp": nc.sync}
        self.sem = {}
        self.cnt = {}
        for e in self.eng:
            self.sem[e] = nc.alloc_semaphore("cnt_" + e)
            self.cnt[e] = 0
        self.same = same_engine_sync
        self.dq = {}
        for q in ("sp", "act", "pool"):
            self.dq[q] = {"sems": [nc.alloc_semaphore(f"dma_{q}_{i}") for i in range(n_dma_sems)],
                          "vals": [0] * n_dma_sems, "next": 0}
        self.seen = {e: {} for e in self.eng}
        self.semobj = {}
        for e in self.eng:
            self.semobj[("e", e)] = self.sem[e]
        self.out_events = []
        self.n_instr = 0
        self._uid = 0
        self._skip_self = False

    def sbuf(self, shape, dtype=F32, name=None):
        self._uid += 1
        nm = f"sb{self._uid}_{name or ''}"
        t = self.nc.alloc_sbuf_tensor(nm, list(shape), dtype)
        return Tile(t.ap(), nm)

    def psum(self, shape, dtype=F32, name=None):
        self._uid += 1
        nm = f"ps{self._uid}_{name or ''}"
        t = self.nc.alloc_psum_tensor(nm, list(shape), dtype)
        return Tile(t.ap(), nm)

    def dram(self, name, shape, dtype=F32, kind="Internal"):
        t = self.nc.dram_tensor(name, list(shape), dtype, kind=kind)
        return Tile(t.ap(), name)

    def view(self, ap, name=""):
        return Tile(ap, name)

    def _wait(self, e, ev):
        if ev is None:
            return
        key, val = ev
        if key == ("e", e) and (not self.same or self._skip_self):
            return
        if self.seen[e].get(key, 0) >= val:
            return
        self.eng[e].wait_ge(self.semobj[key], val)
        self.seen[e][key] = val

    def _deps(self, e, reads, writes):
        reads = [t.bank if t.bank is not None else t for t in reads]
        writes = [t.bank if t.bank is not None else t for t in writes]
        for t in reads:
            self._wait(e, t.last_w)
        for t in writes:
            self._wait(e, t.last_w)
            for ev in t.reads.values():
                self._wait(e, ev)

    def _mark(self, ev, reads, writes):
        reads = [t.bank if t.bank is not None else t for t in reads]
        writes = [t.bank if t.bank is not None else t for t in writes]
        for t in reads:
            t.reads[ev[0]] = ev
        for t in writes:
            t.last_w = ev
            t.reads = {}

    def op(self, e, fn, reads=(), writes=()):
        self._deps(e, reads, writes)
        if e == "pe":
            for t in writes:
                if t.bank is not None:
                    self._wait(e, t.bank.pe_last)
        ins = fn(self.eng[e])
        self.cnt[e] += 1
        ins.then_inc(self.sem[e], 1)
        ev = (("e", e), self.cnt[e])
        if e == "pe":
            for t in writes:
                if t.bank is not None:
                    t.bank.pe_last = ev
        self.seen[e][("e", e)] = self.seen[e].get(("e", e), 0)
        self._mark(ev, reads, writes)
        self.n_instr += 1
        return ev

    def dma(self, q, out, in_, reads=(), writes=(), is_output=False):
        self._deps(q, reads, writes)
        d = self.dq[q]
        i = d["next"]
        d["next"] = (i + 1) % len(d["sems"])
        key = ("d", q, i)
        self.semobj[key] = d["sems"][i]
        if d["vals"][i] > 0:
            self._wait(q, (key, d["vals"][i]))
        ins = self.eng[q].dma_start(out=out, in_=in_)
        d["vals"][i] += 16
        ins.then_inc(d["sems"][i], 16)
        ev = (key, d["vals"][i])
        self._mark(ev, reads, writes)
        if is_output:
            self.out_events.append(ev)
        self.n_instr += 1
        return ev

    def collective(self, kind, src_ap, dst_ap, groups, reads=(), writes=()):
        q = "pool"
        self._deps(q, reads, writes)
        d = self.dq[q]
        i = d["next"]
        d["next"] = (i + 1) % len(d["sems"])
        key = ("d", q, i)
        self.semobj[key] = d["sems"][i]
        if d["vals"][i] > 0:
            self._wait(q, (key, d["vals"][i]))
        ins = self.nc.gpsimd.collective_compute(kind, ALU.bypass, ins=[src_ap], outs=[dst_ap], replica_groups=groups)
        d["vals"][i] += 16
        ins.then_inc(d["sems"][i], 16)
        ev = (key, d["vals"][i])
        self._mark(ev, reads, writes)
        self.n_instr += 1
        return ev

    def finish(self):
        for q, d in self.dq.items():
            for i, v in enumerate(d["vals"]):
                if v > 0:
                    key = ("d", q, i)
                    self._wait("sp", (key, v))
        for e in self.eng:
            if e != "sp" and self.cnt[e] > 0:
                self._wait("sp", (("e", e), self.cnt[e]))


FAST_MM = bool(int(os.environ.get("FAST_MM", "0")))
PE_CHAIN = bool(int(os.environ.get("PE_CHAIN", "1")))


def I(k, e, meth, reads, writes, *a, **kw):
    chain = PE_CHAIN and e == "pe" and meth == "matmul" and kw.get("start") is False
    k._skip_self = chain
    try:
        return k.op(e, lambda eng: getattr(eng, meth)(*a, **kw), reads=reads, writes=writes)
    finally:
        k._skip_self = False


def R32(ap):
    return ap.bitcast(mybir.dt.float32r) if FAST_MM else ap


class Cfg:
    def __init__(self, D=4096, IN_W=11680, ntiles_lat=4, nt_a=512, nt_c=256, nctx=64, E=16, EH=256):
        self.D = D
        self.NKC = D // 128
        self.IN_W = IN_W
        self.NCC_IN = -(-IN_W // 128)
        self.nctx = nctx
        self.nlat = ntiles_lat * nt_a
        self.NTOK = self.nlat + nctx
        self.nt_a = nt_a
        self.nt_c = nt_c
        self.E = E
        self.EH = EH
        self.NHC = 2 * EH // 128
        self.NKD = E * EH // 128

    def tiles(self, nt):
        t = [(i * nt, nt, 0) for i in range(self.nlat // nt)]
        t.append((self.nlat, self.nctx, 1))
        return t


def wlayout(W):
    K, C = W.shape
    nkc = K // 128
    ncc = -(-C // 128)
    if ncc * 128 != C:
        Wp = np.zeros((K, ncc * 128), W.dtype)
        Wp[:, :C] = W
    else:
        Wp = W
    return np.ascontiguousarray(Wp.reshape(nkc, 128, ncc, 128).transpose(2, 1, 0, 3))


def fm(x):
    T, D = x.shape
    return np.ascontiguousarray(x.T.reshape(D // 128, 128, T))


def unfm(xT):
    n, p, T = xT.shape
    return np.ascontiguousarray(xT.reshape(n * p, T).T)


def vec_fm(v):
    sh = v.shape[:-1]
    D = v.shape[-1]
    a = v.reshape(sh + (D // 128, 128))
    a = np.moveaxis(a, -1, 0)
    return np.ascontiguousarray(a)


class TokCommon:
    def __init__(self, k, cfg, nmax):
        self.k = k
        self.cfg = cfg
        self.nmax = nmax
        self.ones = k.sbuf([128, 128], name="ones")
        I(k, "dve", "memset", [], [self.ones], self.ones[:], 1.0)
        self.sq = [k.sbuf([128, nmax], name=f"sq{i}") for i in range(2)]
        self.rstd = k.sbuf([128, nmax], name="rstd")
        self.banks = [k.psum([128, 512], name=f"bank{i}") for i in range(8)]
        self.wbuf = [k.sbuf([128, max(cfg.NKC, cfg.NKD) if hasattr(cfg, "NKD") else cfg.NKC, 128], name=f"wbuf{i}") for i in range(3)]
        self.wi = 0
        self.qs = ["sp", "act", "pool"]
        self.qi = 0

    def nextq(self):
        q = self.qs[self.qi % 2]
        self.qi += 1
        return q

    def load_w(self, src_ap, nk):
        k = self.k
        w = self.wbuf[self.wi % 3]
        self.wi += 1
        k.dma(self.nextq(), R32(w[:, 0:nk, :]), R32(src_ap), writes=[w])
        return w

    def rms_stats(self, X, nk, n, D, eps=1e-6):
        k = self.k
        ss = self.banks[7]
        for kc in range(nk):
            sq = self.sq[kc % 2]
            I(k, "act", "activation", [X], [sq], out=sq[:, :n], in_=X[:, kc, :n], func=AF.Square)
            I(k, "pe", "matmul", [self.ones, sq], [ss], ss[:, :n], lhsT=self.ones[:], rhs=sq[:, :n],
              start=(kc == 0), stop=(kc == nk - 1))
        I(k, "act", "activation", [ss], [self.rstd], out=self.rstd[:, :n], in_=ss[:, :n], func=AF.Sqrt,
          scale=1.0 / D, bias=self.epst[:, 0:1])
        I(k, "dve", "reciprocal", [self.rstd], [self.rstd], out=self.rstd[:, :n], in_=self.rstd[:, :n])

    def norm_mod(self, X, O, nk, n, D, gs, sh):
        k = self.k
        self.rms_stats(X, nk, n, D)
        for kc in range(nk):
            I(k, "dve", "scalar_tensor_tensor", [X, self.rstd, self.prm], [O], out=R32(O[:, kc, :n]), in0=X[:, kc, :n],
              scalar=gs[:, kc:kc + 1], in1=self.rstd[:, :n], op0=ALU.mult, op1=ALU.mult)
            I(k, "act", "activation", [O, self.prm], [O], out=R32(O[:, kc, :n]), in_=O[:, kc, :n], func=AF.Identity,
              bias=sh[:, kc:kc + 1], scale=1.0)


def build_P0(D, ncc_core, L, R=3):
    k = KB()
    nkc = D // 128
    w = k.dram("w", [L, ncc_core, 128, nkc, 128], kind="ExternalInput")
    cT = k.dram("cT", [128, nkc, R], kind="ExternalInput")
    b = k.dram("b", [128, L, ncc_core], kind="ExternalInput")
    out = k.dram("out", [128, L, ncc_core, R], kind="ExternalOutput")
    cs = k.sbuf([128, nkc, R])
    sg = k.sbuf([128, nkc, R])
    bs = k.sbuf([128, L, ncc_core])
    os_ = k.sbuf([128, L, ncc_core, R])
    k.dma("sp", cs[:], cT[:], writes=[cs])
    k.dma("act", bs[:], b[:], writes=[bs])
    I(k, "act", "activation", [cs], [sg], out=sg[:], in_=cs[:], func=AF.Sigmoid)
    I(k, "dve", "tensor_tensor", [cs, sg], [cs], out=cs[:], in0=cs[:], in1=sg[:], op=ALU.mult)
    wb = [k.sbuf([128, nkc, 128], name=f"p0w{i}") for i in range(3)]
    banks = [k.psum([128, 512], name=f"p0b{i}") for i in range(2)]
    it = 0
    for l in range(L):
        for cc in range(ncc_core):
            wt = wb[it % 3]
            k.dma(["sp", "act"][it % 2], wt[:], w[l, cc], writes=[wt])
            ps = banks[it % 2]
            for kc in range(nkc):
                I(k, "pe", "matmul", [wt, cs], [ps], ps[:, 0:R], lhsT=wt[:, kc, :], rhs=cs[:, kc, :],
                  start=(kc == 0), stop=(kc == nkc - 1))
            I(k, "dve", "tensor_scalar", [ps, bs], [os_], out=os_[:, l, cc, :], in0=ps[:, 0:R],
              scalar1=bs[:, l, cc:cc + 1], scalar2=None, op0=ALU.add)
            it += 1
    k.dma("sp", out[:], os_[:], reads=[os_], writes=[out], is_output=True)
    k.finish()
    return k


def build_A(cfg):
    k = KB()
    NKC, NTOK, NCC, D = cfg.NKC, cfg.NTOK, cfg.NCC_IN, cfg.D
    xT = k.dram("xT", [NKC, 128, NTOK], kind="ExternalInput")
    w = k.dram("w", [NCC, 128, NKC, 128], kind="ExternalInput")
    prm = k.dram("prm", [128, 2, 3, NKC], kind="ExternalInput")
    zT = k.dram("zT", [NCC, 128, NTOK], kind="ExternalOutput")
    nmax = cfg.nt_a
    tc = TokCommon(k, cfg, nmax)
    tc.prm = k.sbuf([128, 2, 3, NKC], name="prm")
    tc.epst = k.sbuf([128, 1], name="eps")
    I(k, "dve", "memset", [], [tc.epst], tc.epst[:], 1e-6)
    k.dma("sp", tc.prm[:], prm[:], writes=[tc.prm])
    for ty in range(2):
        I(k, "dve", "scalar_tensor_tensor", [tc.prm], [tc.prm], out=tc.prm[:, ty, 1, :], in0=tc.prm[:, ty, 1, :],
          scalar=1.0, in1=tc.prm[:, ty, 2, :], op0=ALU.add, op1=ALU.mult)
    X = k.sbuf([128, NKC, nmax], name="X")
    zs = [k.sbuf([128, nmax], name=f"zs{i}") for i in range(3)]
    it = 0
    for (t0, n, ty) in cfg.tiles(cfg.nt_a):
        k.dma("sp", R32(X[:, :, :n]), R32(xT[:, :, t0:t0 + n].rearrange("c p t -> p c t")), writes=[X])
        tc.norm_mod(X, X, NKC, n, D, tc.prm[:, ty, 1, :], tc.prm[:, ty, 0, :])
        for cc in range(NCC):
            wt = tc.load_w(w[cc], NKC)
            ps = tc.banks[it % 4]
            for kc in range(NKC):
                I(k, "pe", "matmul", [wt, X], [ps], ps[:, :n], lhsT=R32(wt[:, kc, :]), rhs=R32(X[:, kc, :n]),
                  start=(kc == 0), stop=(kc == NKC - 1))
            z = zs[it % 3]
            if it % 2 == 0:
                I(k, "act", "activation", [ps], [z], out=z[:, :n], in_=ps[:, :n], func=AF.Copy)
            else:
                I(k, "dve", "tensor_copy", [ps], [z], out=z[:, :n], in_=ps[:, :n])
            k.dma("pool", zT[cc, :, t0:t0 + n], z[:, :n], reads=[z], is_output=True)
            it += 1
    k.finish()
    return k


def build_C(cfg, final=False):
    k = KB()
    NKC, NTOK, D, E, NHC, NKD = cfg.NKC, cfg.NTOK, cfg.D, cfg.E, cfg.NHC, cfg.NKD
    NG = 4
    NR = NG + E
    xT = k.dram("xT", [NKC, 128, NTOK], kind="ExternalInput")
    yT = k.dram("yT", [NKC, 128, NTOK], kind="ExternalInput")
    w_out = k.dram("w_out", [NKC, 128, NKC, 128], kind="ExternalInput")
    prm = k.dram("prm", [128, 2, 6, NKC], kind="ExternalInput")
    wcf = k.dram("wcf", [128, NKC, NR], kind="ExternalInput")
    bcf = k.dram("bcf", [128, NR], kind="ExternalInput")
    w_gu = k.dram("w_gu", [E, NHC, 128, NKC, 128], kind="ExternalInput")
    w_dn = k.dram("w_dn", [NKC, 128, NKD, 128], kind="ExternalInput")
    sel = k.dram("sel", [E, E, 128], kind="ExternalInput")
    idn = k.dram("idn", [128, 128], kind="ExternalInput")
    xo = k.dram("xo", [NKC, 128, NTOK], kind="ExternalOutput")
    nmax = cfg.nt_c
    tc = TokCommon(k, cfg, nmax)
    tc.prm = k.sbuf([128, 2, 6, NKC], name="prm")
    tc.epst = k.sbuf([128, 1], name="eps")
    I(k, "dve", "memset", [], [tc.epst], tc.epst[:], 1e-6)
    k.dma("sp", tc.prm[:], prm[:], writes=[tc.prm])
    WCF = k.sbuf([128, NKC, NR], name="WCF")
    BCF = k.sbuf([128, NR], name="BCF")
    SEL = k.sbuf([E, E, 128], name="SEL")
    IDN = k.sbuf([128, 128], name="IDN")
    k.dma("act", WCF[:], wcf[:], writes=[WCF])
    k.dma("act", BCF[:], bcf[:], writes=[BCF])
    k.dma("pool", SEL[:], sel[:], writes=[SEL])
    k.dma("pool", IDN[:], idn[:], writes=[IDN])
    for ty in range(2):
        I(k, "dve", "scalar_tensor_tensor", [tc.prm], [tc.prm], out=tc.prm[:, ty, 2, :], in0=tc.prm[:, ty, 2, :],
          scalar=1.0, in1=tc.prm[:, ty, 4, :], op0=ALU.add, op1=ALU.mult)
    X = k.sbuf([128, NKC, nmax], name="X")
    Y = k.sbuf([128, NKC, nmax], name="Y")
    AT = k.sbuf([128, NKD, nmax], name="AT")
    GT = k.sbuf([E, nmax], name="GT")
    t1 = [k.sbuf([128, nmax], name=f"t1_{i}") for i in range(2)]
    def st(shape, nm):
        return k.sbuf(shape, name=nm)
    Lg = st([128, NR], "Lg"); gmax = st([128, 1], "gmax"); ngmax = st([128, 1], "ngmax")
    ohg = st([128, NG, 1], "ohg"); eg = st([128, NG], "eg"); sume = st([128, 1], "sume"); gprob = st([128, 1], "gprob")
    prod = st([128, NG, 4], "prod"); lsel = st([128, 4], "lsel"); m1 = st([128, 1], "m1"); mk1 = st([128, 4], "mk1")
    le2 = st([128, 4], "le2"); m2 = st([128, 1], "m2"); mk2 = st([128, 4], "mk2"); dd = st([128, 1], "dd")
    e2 = st([128, 1], "e2"); w1 = st([128, 1], "w1"); w2 = st([128, 1], "w2"); gin = st([128, 1, 4], "gin")
    gates = st([128, NG, 4], "gates")
    HB = tc.banks[0:4]
    AB = tc.banks[4:6]
    GB = tc.banks[6]
    RB = tc.banks[7]
    ai = 0
    for (t0, n, ty) in cfg.tiles(cfg.nt_c):
        P = tc.prm
        k.dma("sp", X[:, :, :n], xT[:, :, t0:t0 + n].rearrange("c p t -> p c t"), writes=[X])
        k.dma("act", Y[:, :, :n], yT[:, :, t0:t0 + n].rearrange("c p t -> p c t"), writes=[Y])
        for dc in range(NKC):
            wt = tc.load_w(w_out[dc], NKC)
            ps = AB[ai % 2]; ai += 1
            for kc in range(NKC):
                I(k, "pe", "matmul", [wt, Y], [ps], ps[:, :n], lhsT=R32(wt[:, kc, :]), rhs=R32(Y[:, kc, :n]),
                  start=(kc == 0), stop=(kc == NKC - 1))
            I(k, "dve", "scalar_tensor_tensor", [ps, X, P], [X], out=X[:, dc, :n], in0=ps[:, :n],
              scalar=P[:, ty, 0, dc:dc + 1], in1=X[:, dc, :n], op0=ALU.mult, op1=ALU.add)
        tc.norm_mod(X, Y, NKC, n, D, P[:, ty, 2, :], P[:, ty, 1, :])
        for tb in range(-(-n // 128)):
            nb = min(128, n - tb * 128)
            for kc in range(NKC):
                I(k, "pe", "matmul", [Y, WCF], [RB], RB[:nb, 0:NR], lhsT=Y[:, kc, tb * 128:tb * 128 + nb],
                  rhs=WCF[:, kc, :], start=(kc == 0), stop=(kc == NKC - 1))
            V = lambda *a, **kw: I(k, "dve", *a, **kw)
            V("tensor_tensor", [RB, BCF], [Lg], out=Lg[:nb, :], in0=RB[:nb, 0:NR], in1=BCF[:nb, :], op=ALU.add)
            V("tensor_reduce", [Lg], [gmax], out=gmax[:nb, :], in_=Lg[:nb, 0:NG], op=ALU.max, axis=AX.X)
            V("tensor_scalar", [gmax], [ngmax], out=ngmax[:nb, :], in0=gmax[:nb, :], scalar1=-1.0, scalar2=None, op0=ALU.mult)
            V("tensor_scalar", [Lg, gmax], [ohg], out=ohg[:nb, :, 0], in0=Lg[:nb, 0:NG], scalar1=gmax[:nb, 0:1],
              scalar2=None, op0=ALU.is_equal)
            I(k, "act", "activation", [Lg, ngmax], [eg, sume], out=eg[:nb, :], in_=Lg[:nb, 0:NG], func=AF.Exp,
              bias=ngmax[:nb, 0:1], scale=1.0, accum_out=sume[:nb, 0:1])
            V("reciprocal", [sume], [gprob], out=gprob[:nb, :], in_=sume[:nb, :])
            V("tensor_tensor", [Lg, ohg], [prod], out=prod[:nb], in0=Lg[:nb, NG:NR].rearrange("p (g e) -> p g e", g=NG),
              in1=ohg[:nb].to_broadcast([nb, NG, 4]), op=ALU.mult)
            V("tensor_reduce", [prod], [lsel], out=lsel[:nb, :], in_=prod[:nb].rearrange("p g e -> p e g"), op=ALU.add, axis=AX.X)
            V("tensor_reduce", [lsel], [m1], out=m1[:nb, :], in_=lsel[:nb, :], op=ALU.max, axis=AX.X)
            V("tensor_scalar", [lsel, m1], [mk1], out=mk1[:nb, :], in0=lsel[:nb, :], scalar1=m1[:nb, 0:1], scalar2=None, op0=ALU.is_equal)
            V("scalar_tensor_tensor", [mk1, lsel], [le2], out=le2[:nb, :], in0=mk1[:nb, :], scalar=-1e30, in1=lsel[:nb, :],
              op0=ALU.mult, op1=ALU.add)
            V("tensor_reduce", [le2], [m2], out=m2[:nb, :], in_=le2[:nb, :], op=ALU.max, axis=AX.X)
            V("tensor_scalar", [le2, m2], [mk2], out=mk2[:nb, :], in0=le2[:nb, :], scalar1=m2[:nb, 0:1], scalar2=None, op0=ALU.is_equal)
            V("tensor_tensor", [m2, m1], [dd], out=dd[:nb, :], in0=m2[:nb, :], in1=m1[:nb, :], op=ALU.subtract)
            I(k, "act", "activation", [dd], [e2], out=e2[:nb, :], in_=dd[:nb, :], func=AF.Exp)
            V("tensor_scalar", [e2], [w1], out=w1[:nb, :], in0=e2[:nb, :], scalar1=1.0, scalar2=None, op0=ALU.add)
            V("reciprocal", [w1], [w1], out=w1[:nb, :], in_=w1[:nb, :])
            V("tensor_tensor", [e2, w1], [w2], out=w2[:nb, :], in0=e2[:nb, :], in1=w1[:nb, :], op=ALU.mult)
            V("tensor_scalar", [mk1, w1], [gin], out=gin[:nb, 0, :], in0=mk1[:nb, :], scalar1=w1[:nb, 0:1], scalar2=None, op0=ALU.mult)
            V("scalar_tensor_tensor", [mk2, w2, gin], [gin], out=gin[:nb, 0, :], in0=mk2[:nb, :], scalar=w2[:nb, 0:1],
              in1=gin[:nb, 0, :], op0=ALU.mult, op1=ALU.add)
            V("tensor_scalar", [gin, gprob], [gin], out=gin[:nb, 0, :], in0=gin[:nb, 0, :], scalar1=gprob[:nb, 0:1], scalar2=None, op0=ALU.mult)
            V("tensor_tensor", [ohg, gin], [gates], out=gates[:nb], in0=ohg[:nb].to_broadcast([nb, NG, 4]),
              in1=gin[:nb].to_broadcast([nb, NG, 4]), op=ALU.mult)
            I(k, "pe", "transpose", [gates, IDN], [RB], RB[:E, 256:256 + nb], gates[:nb].rearrange("p g e -> p (g e)"), IDN[:nb, :nb])
            V("tensor_copy", [RB], [GT], out=GT[:, tb * 128:tb * 128 + nb], in_=RB[:E, 256:256 + nb])
        for e in range(E):
            for hc in range(NHC):
                wt = tc.load_w(w_gu[e, hc], NKC)
                for kc in range(NKC):
                    I(k, "pe", "matmul", [wt, Y], [HB[hc]], HB[hc][:, :n], lhsT=R32(wt[:, kc, :]), rhs=R32(Y[:, kc, :n]),
                      start=(kc == 0), stop=(kc == NKC - 1))
            I(k, "pe", "matmul", [SEL, GT], [GB], GB[:, :n], lhsT=SEL[:, e, :], rhs=GT[:, :n], start=True, stop=True)
            for j in range(NHC // 2):
                I(k, "act", "activation", [HB[j]], [t1[j]], out=t1[j][:, :n], in_=HB[j][:, :n], func=AF.Silu)
                I(k, "dve", "tensor_tensor", [t1[j], HB[NHC // 2 + j]], [t1[j]], out=t1[j][:, :n], in0=t1[j][:, :n],
                  in1=HB[NHC // 2 + j][:, :n], op=ALU.mult)
                I(k, "dve", "tensor_tensor", [t1[j], GB], [AT], out=R32(AT[:, e * (NHC // 2) + j, :n]), in0=t1[j][:, :n],
                  in1=GB[:, :n], op=ALU.mult)
        for dc in range(NKC):
            wt = tc.load_w(w_dn[dc], NKD)
            ps = AB[ai % 2]; ai += 1
            for kc in range(NKD):
                I(k, "pe", "matmul", [wt, AT], [ps], ps[:, :n], lhsT=R32(wt[:, kc, :]), rhs=R32(AT[:, kc, :n]),
                  start=(kc == 0), stop=(kc == NKD - 1))
            I(k, "dve", "scalar_tensor_tensor", [ps, X, P], [X], out=X[:, dc, :n], in0=ps[:, :n],
              scalar=P[:, ty, 3, dc:dc + 1], in1=X[:, dc, :n], op0=ALU.mult, op1=ALU.add)
        if final:
            tc.rms_stats(X, NKC, n, D)
            for kc in range(NKC):
                I(k, "dve", "scalar_tensor_tensor", [X, tc.rstd, P], [X], out=X[:, kc, :n], in0=X[:, kc, :n],
                  scalar=P[:, ty, 5, kc:kc + 1], in1=tc.rstd[:, :n], op0=ALU.mult, op1=ALU.mult)
        k.dma("pool", xo[:, :, t0:t0 + n].rearrange("c p t -> p c t"), X[:, :, :n], reads=[X], is_output=True)
    k.finish()
    return k


def seq_segments(Tc, Tl, SEG):
    segs = [(0, Tc, 0, Tc)]
    for s in range(Tc, Tc + Tl, SEG):
        segs.append((s, min(s + SEG, Tc + Tl), Tc, Tc + Tl))
    return segs


def gelu_tanh(k, out, x, tmp, n):
    I(k, "dve", "tensor_tensor", [x], [tmp], out=tmp[:, :n], in0=x[:, :n], in1=x[:, :n], op=ALU.mult)
    I(k, "dve", "tensor_scalar", [tmp], [tmp], out=tmp[:, :n], in0=tmp[:, :n], scalar1=0.044715, scalar2=1.0,
      op0=ALU.mult, op1=ALU.add)
    I(k, "dve", "tensor_tensor", [tmp, x], [tmp], out=tmp[:, :n], in0=tmp[:, :n], in1=x[:, :n], op=ALU.mult)
    I(k, "act", "activation", [tmp], [tmp], out=tmp[:, :n], in_=tmp[:, :n], func=AF.Sigmoid, scale=1.5957691216057308)
    I(k, "dve", "tensor_tensor", [tmp, x], [out], out=out[:, :n], in0=tmp[:, :n], in1=x[:, :n], op=ALU.mult)


def build_LRU(Tc, Tl, SEG, NB=2):
    k = KB()
    T = Tc + Tl
    zx = k.dram("zx", [NB, 128, T], kind="ExternalInput")
    zy = k.dram("zy", [NB, 128, T], kind="ExternalInput")
    prm = k.dram("prm", [128, NB, 11], kind="ExternalInput")
    wa = k.dram("wa", [128, NB, 2, 128], kind="ExternalInput")
    wx = k.dram("wx", [128, NB, 2, 128], kind="ExternalInput")
    yo = k.dram("yo", [NB, 128, T], kind="ExternalOutput")
    P = k.sbuf([128, NB, 11], name="P"); WA = k.sbuf([128, NB, 2, 128], name="WA"); WX = k.sbuf([128, NB, 2, 128], name="WX")
    k.dma("sp", P[:], prm[:], writes=[P]); k.dma("act", WA[:], wa[:], writes=[WA]); k.dma("pool", WX[:], wx[:], writes=[WX])
    one = k.sbuf([128, 1], name="one")
    I(k, "dve", "memset", [], [one], one[:], 1.0)
    C8 = k.sbuf([128, NB, 2], name="C8"); C16 = k.sbuf([128, NB, 2], name="C16")
    I(k, "act", "activation", [P], [C8], out=C8[:], in_=P[:, :, 9:11], func=AF.Exp, scale=-1.0)
    I(k, "act", "activation", [C8, one], [C8], out=C8[:], in_=C8[:], func=AF.Ln, bias=one[:, 0:1], scale=1.0)
    I(k, "dve", "tensor_scalar", [C8], [C16], out=C16[:], in0=C8[:], scalar1=-16.0, scalar2=None, op0=ALU.mult)
    I(k, "dve", "tensor_scalar", [C8], [C8], out=C8[:], in0=C8[:], scalar1=-8.0, scalar2=None, op0=ALU.mult)
    OF = k.sbuf([128, T], name="OF")
    XH = k.sbuf([128, SEG + 3], name="XH"); XC = k.sbuf([128, SEG], name="XC")
    Rr = k.sbuf([128, SEG], name="Rr"); Ii = k.sbuf([128, SEG], name="Ii"); A2 = k.sbuf([128, SEG], name="A2")
    YG = k.sbuf([128, SEG], name="YG"); OB = k.sbuf([128, SEG], name="OB"); TM = k.sbuf([128, SEG], name="TM")
    hst = k.sbuf([128, 1], name="hst")
    banks = [k.psum([128, 512], name=f"lb{i}") for i in range(4)]
    segs = seq_segments(Tc, Tl, SEG)
    bi = 0

    def prep(blk, d, seg):
        nonlocal bi
        s0, s1, q0, q1 = seg
        n = s1 - s0
        lo, hi = max(s0 - 2, q0), min(s1 + 1, q1)
        I(k, "pool", "memset", [], [XH], XH[:, :n + 3], 0.0)
        k.dma("sp", XH[:, lo - (s0 - 2):hi - (s0 - 2)], zx[blk, :, lo:hi], writes=[XH])
        I(k, "dve", "tensor_scalar", [XH, P], [XC], out=XC[:, :n], in0=XH[:, 0:n], scalar1=P[:, blk, 0:1],
          scalar2=P[:, blk, 4:5], op0=ALU.mult, op1=ALU.add)
        for j in range(1, 4):
            I(k, "dve", "scalar_tensor_tensor", [XH, P, XC], [XC], out=XC[:, :n], in0=XH[:, j:j + n],
              scalar=P[:, blk, j:j + 1], in1=XC[:, :n], op0=ALU.mult, op1=ALU.add)
        for c0 in range(0, n, 512):
            cn = min(512, n - c0)
            pa = banks[bi % 4]; px = banks[(bi + 1) % 4]; bi += 2
            I(k, "pe", "matmul", [WA, XC], [pa], pa[:, :cn], lhsT=WA[:, blk, d, :], rhs=XC[:, c0:c0 + cn], start=True, stop=True)
            I(k, "pe", "matmul", [WX, XC], [px], px[:, :cn], lhsT=WX[:, blk, d, :], rhs=XC[:, c0:c0 + cn], start=True, stop=True)
            I(k, "act", "activation", [pa, P], [Rr], out=Rr[:, c0:c0 + cn], in_=pa[:, :cn], func=AF.Sigmoid,
              bias=P[:, blk, 5 + d:6 + d], scale=1.0)
            I(k, "act", "activation", [px, P], [Ii], out=Ii[:, c0:c0 + cn], in_=px[:, :cn], func=AF.Sigmoid,
              bias=P[:, blk, 7 + d:8 + d], scale=1.0)
        I(k, "act", "activation", [Rr, C16], [A2], out=A2[:, :n], in_=Rr[:, :n], func=AF.Exp, scale=C16[:, blk, d:d + 1])
        I(k, "act", "activation", [Rr, C8], [Rr], out=Rr[:, :n], in_=Rr[:, :n], func=AF.Exp, scale=C8[:, blk, d:d + 1])
        I(k, "act", "activation", [A2, one], [A2], out=A2[:, :n], in_=A2[:, :n], func=AF.Sqrt, scale=-1.0, bias=one[:, 0:1])
        I(k, "dve", "tensor_tensor", [A2, Ii], [A2], out=A2[:, :n], in0=A2[:, :n], in1=Ii[:, :n], op=ALU.mult)
        I(k, "dve", "tensor_tensor", [A2, XC], [A2], out=A2[:, :n], in0=A2[:, :n], in1=XC[:, :n], op=ALU.mult)
        return n

    for blk in range(NB):
        I(k, "dve", "memset", [], [hst], hst[:], 0.0)
        for seg in segs:
            n = prep(blk, 0, seg)
            s0 = seg[0]
            I(k, "dve", "tensor_tensor_scan", [Rr, A2, hst], [OF], out=OF[:, s0:s0 + n], data0=Rr[:, :n], data1=A2[:, :n],
              initial=hst[:, 0:1], op0=ALU.mult, op1=ALU.add)
            I(k, "dve", "tensor_copy", [OF], [hst], out=hst[:], in_=OF[:, s0 + n - 1:s0 + n])
        I(k, "dve", "memset", [], [hst], hst[:], 0.0)
        for seg in [segs[0]] + segs[1:][::-1]:
            n = prep(blk, 1, seg)
            s0 = seg[0]
            k.dma("act", YG[:, :n], zy[blk, :, s0:s0 + n], writes=[YG])
            I(k, "dve", "tensor_tensor_scan", [Rr, A2, hst], [OB], out=OB[:, :n][:, ::-1], data0=Rr[:, :n][:, ::-1],
              data1=A2[:, :n][:, ::-1], initial=hst[:, 0:1], op0=ALU.mult, op1=ALU.add)
            I(k, "dve", "tensor_copy", [OB], [hst], out=hst[:], in_=OB[:, 0:1])
            gelu_tanh(k, Ii, YG, TM, n)
            I(k, "dve", "tensor_tensor", [OB, OF], [OB], out=OB[:, :n], in0=OB[:, :n], in1=OF[:, s0:s0 + n], op=ALU.add)
            I(k, "dve", "tensor_tensor", [OB, Ii], [OB], out=OB[:, :n], in0=OB[:, :n], in1=Ii[:, :n], op=ALU.mult)
            k.dma("pool", yo[blk, :, s0:s0 + n], OB[:, :n], reads=[OB], is_output=True)
    k.finish()
    return k


POOL_WINDOWS = (2, 4, 8, 16)


def build_POOL(groups):
    k = KB()
    NG = len(groups)
    zin = [k.dram(f"zp{i}", [8, 128, nl, lw + 16], kind="ExternalInput") for i, (nl, lw) in enumerate(groups)]
    inv = [k.dram(f"inv{i}", [128, 4, lw], kind="ExternalInput") for i, (nl, lw) in enumerate(groups)]
    lin = k.dram("lin", [128, 4, 2, 256], kind="ExternalInput")
    scl = k.dram("scl", [128, 8], kind="ExternalInput")
    yout = [k.dram(f"yp{i}", [8, 128, nl, lw], kind="ExternalOutput") for i, (nl, lw) in enumerate(groups)]
    LIN = k.sbuf([128, 4, 2, 256], name="LIN"); SCL = k.sbuf([128, 8], name="SCL")
    k.dma("sp", LIN[:], lin[:], writes=[LIN]); k.dma("act", SCL[:], scl[:], writes=[SCL])
    banks = [k.psum([128, 512], name=f"pb{i}") for i in range(4)]
    bi = 0
    for gi_, (NL, LW) in enumerate(groups):
        LP = LW + 16
        INV = k.sbuf([128, 4, 1, LW], name=f"INV{gi_}")
        k.dma("act", INV[:, :, 0, :], inv[gi_][:], writes=[INV])
        XH = [k.sbuf([128, NL, LP], name=f"XH{gi_}_{i}") for i in range(2)]
        SA = k.sbuf([128, NL, LP], name=f"SA{gi_}"); SB = k.sbuf([128, NL, LP], name=f"SB{gi_}")
        M = [k.sbuf([128, NL, LW], name=f"M{gi_}_{i}") for i in range(2)]
        O = [k.sbuf([128, 512], name=f"O{gi_}_{i}") for i in range(2)]
        ntok = NL * LW
        for g, w in enumerate(POOL_WINDOWS):
            for ic in range(2):
                cb = 2 * g + ic
                k.dma(["sp", "act"][ic], XH[ic][:], zin[gi_][cb], writes=[XH[ic]])
                src = XH[ic]; L = LP; sh = 1
                bufs = [SA, SB]; bsel = 0
                while sh < w:
                    dst = bufs[bsel]; bsel ^= 1
                    I(k, "dve", "tensor_tensor", [src], [dst], out=dst[:, :, 0:L - sh], in0=src[:, :, 0:L - sh],
                      in1=src[:, :, sh:L], op=ALU.add)
                    src = dst; L -= sh; sh *= 2
                o = 8 - w // 2
                I(k, "dve", "tensor_tensor", [src, INV], [M[ic]], out=M[ic][:], in0=src[:, :, o:o + LW],
                  in1=INV[:, g].to_broadcast([128, NL, LW]), op=ALU.mult)
                I(k, "dve", "tensor_tensor", [M[ic], XH[ic]], [M[ic]], out=M[ic][:], in0=M[ic][:], in1=XH[ic][:, :, 8:8 + LW],
                  op=ALU.subtract)
            for oc in range(2):
                for c0 in range(0, ntok, 512):
                    cn = min(512, ntok - c0)
                    ps = banks[bi % 4]; ot = O[bi % 2]; bi += 1
                    for ic in range(2):
                        I(k, "pe", "matmul", [LIN, M[ic]], [ps], ps[:, :cn], lhsT=LIN[:, g, ic, oc * 128:(oc + 1) * 128],
                          rhs=M[ic][:].rearrange("p l t -> p (l t)")[:, c0:c0 + cn], start=(ic == 0), stop=(ic == 1))
                    I(k, "act", "activation", [ps, SCL], [ot], out=ot[:, :cn], in_=ps[:, :cn], func=AF.Copy,
                      scale=SCL[:, 2 * g + oc:2 * g + oc + 1])
                    k.dma("pool", yout[gi_][2 * g + oc].rearrange("p l t -> p (l t)")[:, c0:c0 + cn], ot[:, :cn], reads=[ot], is_output=True)
    k.finish()
    return k


def pool_invcount(n, positions):
    out = np.zeros((4, len(positions)), np.float32)
    for g, w in enumerate(POOL_WINDOWS):
        t = np.asarray(positions)
        lo = np.clip(t - w // 2, 0, n); hi = np.clip(t + w // 2, 0, n)
        out[g] = 1.0 / (hi - lo)
    return out


HG_C = 32


def build_HG(Tc, Tl, SEG, NH=2, L=4):
    k = KB()
    T = Tc + Tl
    C = HG_C
    zd0 = k.dram("zd0", [NH, 3, 128, T], kind="ExternalInput")
    zfb = k.dram("zfb", [NH, 128, T], kind="ExternalInput")
    zg = k.dram("zg", [NH, 128, T], kind="ExternalInput")
    lbp = k.dram("lbp", [128, NH, 2, L], kind="ExternalInput")
    lmask = k.dram("lmask", [128, L], kind="ExternalInput")
    gn = k.dram("gn", [128, 1], kind="ExternalInput")
    msk = k.dram("msk", [C, C], kind="ExternalInput")
    idn = k.dram("idn", [128, 128], kind="ExternalInput")
    yo = k.dram("yo", [NH, 128, T], kind="ExternalOutput")
    LBP = k.sbuf([128, NH, 2, L], name="LBP"); LM = k.sbuf([128, 1, 1, L], name="LM"); GN = k.sbuf([128, 1], name="GN")
    MSK = k.sbuf([C, C], name="MSK"); IDN = k.sbuf([128, 128], name="IDN")
    k.dma("sp", LBP[:], lbp[:], writes=[LBP]); k.dma("sp", LM[:, 0, 0, :], lmask[:], writes=[LM]); k.dma("sp", GN[:], gn[:], writes=[GN])
    k.dma("act", MSK[:], msk[:], writes=[MSK]); k.dma("act", IDN[:], idn[:], writes=[IDN])
    ones = k.sbuf([128, max(SEG, 128)], name="ones")
    I(k, "dve", "memset", [], [ones], ones[:], 1.0)
    LB = k.sbuf([128, NH, 2], name="LB"); OML = k.sbuf([128, NH, 2], name="OML")
    mx = k.sbuf([128, NH, 2], name="mx"); sm = k.sbuf([128, NH, 2], name="sm")
    EX = k.sbuf([128, NH, 2, L], name="EX")
    I(k, "dve", "tensor_reduce", [LBP], [mx], out=mx[:], in_=LBP[:], op=ALU.max, axis=AX.X)
    I(k, "dve", "tensor_tensor", [LBP, mx], [EX], out=EX[:], in0=LBP[:], in1=mx[:].unsqueeze(3).to_broadcast([128, NH, 2, L]), op=ALU.subtract)
    I(k, "act", "activation", [EX], [EX], out=EX[:], in_=EX[:], func=AF.Exp)
    I(k, "dve", "tensor_reduce", [EX], [sm], out=sm[:], in_=EX[:], op=ALU.add, axis=AX.X)
    I(k, "dve", "tensor_tensor", [EX, LM], [EX], out=EX[:], in0=EX[:], in1=LM[:].to_broadcast([128, NH, 2, L]), op=ALU.mult)
    I(k, "dve", "tensor_reduce", [EX], [LB], out=LB[:], in_=EX[:], op=ALU.add, axis=AX.X)
    I(k, "dve", "reciprocal", [sm], [sm], out=sm[:], in_=sm[:])
    I(k, "dve", "tensor_tensor", [LB, sm], [LB], out=LB[:], in0=LB[:], in1=sm[:], op=ALU.mult)
    I(k, "dve", "tensor_scalar", [LB], [OML], out=OML[:], in0=LB[:], scalar1=-1.0, scalar2=1.0, op0=ALU.mult, op1=ALU.add)

    NCH = SEG // C
    OD = [k.sbuf([128, T], name=f"OD{d}") for d in range(2)]
    ZQ = k.sbuf([128, SEG], name="ZQ"); ZF = k.sbuf([128, SEG], name="ZF"); ZV = k.sbuf([128, SEG], name="ZV")
    RV = [k.sbuf([128, SEG], name=f"RV{i}") for i in range(3)]
    KK = k.sbuf([128, SEG], name="KK"); CUM = k.sbuf([128, SEG], name="CUM"); DD = k.sbuf([128, SEG], name="DD")
    E2 = k.sbuf([128, SEG], name="E2")
    BASE = k.sbuf([128, NCH], name="BASE"); EM = k.sbuf([128, NCH], name="EM"); ETOT = k.sbuf([128, NCH], name="ETOT")
    S = k.sbuf([128, 128], name="S"); SP = [k.sbuf([128, 128], name=f"SP{i}") for i in range(2)]
    KV = [k.sbuf([C, 256], name=f"KV{i}") for i in range(2)]
    SCM = [k.sbuf([C, C], name=f"SCM{i}") for i in range(2)]
    cst = k.sbuf([128, 1], name="cst")
    TP = [k.psum([128, 512], name=f"TP{i}") for i in range(2)]
    SC = [k.psum([128, 512], name=f"SC{i}") for i in range(2)]
    DP = [k.psum([128, 512], name=f"DP{i}") for i in range(2)]
    OP = [k.psum([128, 512], name=f"OP{i}") for i in range(2)]
    segs = seq_segments(Tc, Tl, SEG)
    ci = 0
    opi = 0
    for h in range(NH):
        for d in range(2):
            I(k, "dve", "memset", [], [S], S[:], 0.0)
            I(k, "dve", "memset", [], [cst], cst[:], 0.0)
            for (s0, s1, q0, q1) in segs:
                n = s1 - s0
                nch = n // C
                if d == 0:
                    k.dma("sp", ZQ[:, :n], zd0[h, 0, :, s0:s1], writes=[ZQ])
                    k.dma("act", ZF[:, :n], zd0[h, 1, :, s0:s1], writes=[ZF])
                    k.dma("pool", ZV[:, :n], zd0[h, 2, :, s0:s1], writes=[ZV])
                else:
                    g0 = q0 + (q1 - s1); g1 = q0 + (q1 - s0)
                    k.dma("sp", RV[0][:, :n], zd0[h, 0, :, g0:g1], writes=[RV[0]])
                    k.dma("act", RV[1][:, :n], zfb[h, :, g0:g1], writes=[RV[1]])
                    k.dma("pool", RV[2][:, :n], zd0[h, 2, :, g0:g1], writes=[RV[2]])
                    for rv, dst in zip(RV, (ZQ, ZF, ZV)):
                        I(k, "pool", "tensor_copy", [rv], [dst], out=dst[:, :n], in_=rv[:, :n][:, ::-1])
                I(k, "act", "activation", [ZQ], [ZQ], out=ZQ[:, :n], in_=ZQ[:, :n], func=AF.Silu)
                I(k, "act", "activation", [ZF], [ZF], out=ZF[:, :n], in_=ZF[:, :n], func=AF.Sigmoid)
                I(k, "dve", "tensor_scalar", [ZF, OML, LB], [ZF], out=ZF[:, :n], in0=ZF[:, :n], scalar1=OML[:, h, d:d + 1],
                  scalar2=LB[:, h, d:d + 1], op0=ALU.mult, op1=ALU.add)
                I(k, "dve", "tensor_scalar", [ZF], [KK], out=KK[:, :n], in0=ZF[:, :n], scalar1=-1.0, scalar2=1.0, op0=ALU.mult, op1=ALU.add)
                I(k, "act", "activation", [ZF], [ZF], out=ZF[:, :n], in_=ZF[:, :n], func=AF.Ln)
                I(k, "dve", "tensor_tensor_scan", [ones, ZF], [CUM], out=CUM[:, :n], data0=ones[:, :n], data1=ZF[:, :n],
                  initial=0.0, op0=ALU.mult, op1=ALU.add)
                C3 = CUM[:, :n].rearrange("p (c t) -> p c t", t=C)
                D3 = DD[:, :n].rearrange("p (c t) -> p c t", t=C)
                I(k, "dve", "memset", [], [BASE], BASE[:, 0:1], 0.0)
                if nch > 1:
                    I(k, "dve", "tensor_copy", [CUM], [BASE], out=BASE[:, 1:nch], in_=C3[:, 0:nch - 1, C - 1])
                I(k, "dve", "tensor_tensor", [CUM, BASE], [EM], out=EM[:, :nch], in0=C3[:, :, C // 2 - 1], in1=BASE[:, :nch], op=ALU.subtract)
                I(k, "dve", "tensor_tensor", [CUM, BASE], [ETOT], out=ETOT[:, :nch], in0=C3[:, :, C - 1], in1=BASE[:, :nch], op=ALU.subtract)
                I(k, "act", "activation", [EM], [EM], out=EM[:, :nch], in_=EM[:, :nch], func=AF.Exp)
                I(k, "act", "activation", [ETOT], [ETOT], out=ETOT[:, :nch], in_=ETOT[:, :nch], func=AF.Exp)
                I(k, "dve", "tensor_tensor", [CUM], [DD], out=D3, in0=C3, in1=C3[:, :, C // 2 - 1:C // 2].to_broadcast([128, nch, C]), op=ALU.subtract)
                I(k, "act", "activation", [DD], [E2], out=E2[:, :n], in_=DD[:, :n], func=AF.Exp, scale=-1.0)
                I(k, "act", "activation", [DD], [DD], out=DD[:, :n], in_=DD[:, :n], func=AF.Exp)
                I(k, "dve", "scalar_tensor_tensor", [ZQ, DD], [ZQ], out=ZQ[:, :n], in0=ZQ[:, :n], scalar=float(128 ** -0.5), in1=DD[:, :n],
                  op0=ALU.mult, op1=ALU.mult)
                I(k, "dve", "tensor_tensor", [KK, E2], [KK], out=KK[:, :n], in0=KK[:, :n], in1=E2[:, :n], op=ALU.mult)
                for c in range(nch):
                    cs = slice(c * C, (c + 1) * C)
                    tp = TP[ci % 2]; sc = SC[ci % 2]; dp = DP[ci % 2]; kv = KV[ci % 2]; scm = SCM[ci % 2]; sp = SP[ci % 2]
                    if c % 16 == 0:
                        op_ = OP[opi % 2]; opi += 1
                    oc = (c % 16) * C
                    ci += 1
                    I(k, "pe", "transpose", [KK, IDN], [tp], tp[:C, 0:128], KK[:, cs], IDN[:])
                    I(k, "pe", "transpose", [ZV, IDN], [tp], tp[:C, 128:256], ZV[:, cs], IDN[:])
                    I(k, "act", "activation", [tp], [kv], out=kv[:], in_=tp[:C, 0:256], func=AF.Copy)
                    I(k, "pe", "matmul", [KK, ZQ], [sc], sc[:C, 0:C], lhsT=KK[:, cs], rhs=ZQ[:, cs], start=True, stop=True)
                    I(k, "dve", "tensor_tensor", [sc, MSK], [scm], out=scm[:], in0=sc[:C, 0:C], in1=MSK[:], op=ALU.mult)
                    I(k, "pe", "matmul", [kv], [dp], dp[:, 0:128], lhsT=kv[:, 0:128], rhs=kv[:, 128:256], start=True, stop=True)
                    I(k, "dve", "tensor_scalar", [S, EM], [sp], out=sp[:], in0=S[:], scalar1=EM[:, c:c + 1], scalar2=None, op0=ALU.mult)
                    I(k, "pe", "matmul", [kv, scm], [op_], op_[:, oc:oc + C], lhsT=kv[:, 128:256], rhs=scm[:], start=True, stop=False)
                    I(k, "pe", "matmul", [sp, ZQ], [op_], op_[:, oc:oc + C], lhsT=sp[:], rhs=ZQ[:, cs], start=False, stop=True)
                    I(k, "dve", "tensor_scalar", [S, ETOT], [S], out=S[:], in0=S[:], scalar1=ETOT[:, c:c + 1], scalar2=None, op0=ALU.mult)
                    I(k, "dve", "scalar_tensor_tensor", [dp, DD, S], [S], out=S[:], in0=dp[:, 0:128], scalar=DD[:, (c + 1) * C - 1:(c + 1) * C],
                      in1=S[:], op0=ALU.mult, op1=ALU.add)
                    if c % 16 == 15 or c == nch - 1:
                        c0 = (c // 16) * 16 * C
                        w = (c % 16 + 1) * C
                        I(k, "act", "activation", [op_], [OD[d]], out=OD[d][:, s0 + c0:s0 + c0 + w], in_=op_[:, 0:w], func=AF.Copy)
        ZG = ZQ; OS = KK; SQ = CUM; RS = DD; TMP = E2
        for (s0, s1, q0, q1) in segs:
            n = s1 - s0
            f0 = q0 + (q1 - s1); f1 = q0 + (q1 - s0)
            k.dma("sp", ZG[:, :n], zg[h, :, s0:s1], writes=[ZG])
            I(k, "dve", "tensor_tensor", [OD[0], OD[1]], [OS], out=OS[:, :n], in0=OD[0][:, s0:s1], in1=OD[1][:, f0:f1][:, ::-1], op=ALU.add)
            I(k, "act", "activation", [OS], [SQ], out=SQ[:, :n], in_=OS[:, :n], func=AF.Square)
            for c0 in range(0, n, 512):
                cn = min(512, n - c0)
                ps = OP[opi % 2]; opi += 1
                I(k, "pe", "matmul", [ones, SQ], [ps], ps[:, :cn], lhsT=ones[:, 0:128], rhs=SQ[:, c0:c0 + cn], start=True, stop=True)
                I(k, "dve", "tensor_scalar", [ps], [RS], out=RS[:, c0:c0 + cn], in0=ps[:, :cn], scalar1=1.0 / 128, scalar2=1e-6, op0=ALU.mult, op1=ALU.add)
            I(k, "act", "activation", [RS], [RS], out=RS[:, :n], in_=RS[:, :n], func=AF.Sqrt)
            I(k, "dve", "reciprocal", [RS], [RS], out=RS[:, :n], in_=RS[:, :n])
            I(k, "dve", "scalar_tensor_tensor", [OS, GN, RS], [OS], out=OS[:, :n], in0=OS[:, :n], scalar=GN[:, 0:1], in1=RS[:, :n],
              op0=ALU.mult, op1=ALU.mult)
            I(k, "act", "activation", [ZG], [ZG], out=ZG[:, :n], in_=ZG[:, :n], func=AF.Silu)
            I(k, "dve", "tensor_tensor", [OS, ZG], [OS], out=OS[:, :n], in0=OS[:, :n], in1=ZG[:, :n], op=ALU.mult)
            k.dma("pool", yo[h, :, s0:s1], OS[:, :n], reads=[OS], is_output=True)
    k.finish()
    return k


RW_C = 64
import os
SUBDBG = int(os.environ.get('SUBDBG', '9'))


def build_RW(Tc, Tl, SEG, NP=2, dbg=9):
    k = KB()
    T = Tc + Tl
    C = RW_C
    NCH = SEG // C
    zr0 = k.dram("zr0", [NP, 3, 128, T], kind="ExternalInput")
    zr = [zr0, zr0]
    zl = [k.dram(f"zl{d}", [128, T], kind="ExternalInput") for d in range(2)]
    zgd = k.dram("zgd", [2, 128, T], kind="ExternalInput")
    prm = k.dram("prm", [128, NP, 12], kind="ExternalInput")
    mul = k.dram("mul", [128, 4], kind="ExternalInput")
    wup = k.dram("wup", [128, 2, NP, 128], kind="ExternalInput")
    gup = k.dram("gup", [128, 2, NP, 128], kind="ExternalInput")
    idn2 = k.dram("idn2", [128, 256], kind="ExternalInput")
    msk = k.dram("msk", [128, 128], kind="ExternalInput")
    blk = k.dram("blk", [128, 128], kind="ExternalInput")
    yo = k.dram("yo", [NP, 128, T], kind="ExternalOutput")
    P = k.sbuf([128, NP, 12], name="P"); MUL = k.sbuf([128, 4], name="MUL")
    WUP = k.sbuf([128, 2, NP, 128], name="WUP"); GUP = k.sbuf([128, 2, NP, 128], name="GUP")
    IDN2 = k.sbuf([128, 256], name="IDN2"); MS = k.sbuf([128, 128], name="MS"); BLK = k.sbuf([128, 128], name="BLK")
    for i, (dst, src) in enumerate([(P, prm), (MUL, mul), (WUP, wup), (GUP, gup), (IDN2, idn2), (MS, msk), (BLK, blk)]):
        k.dma(["sp", "act", "pool"][i % 3], dst[:], src[:], writes=[dst])
    OMM = k.sbuf([128, NP, 3], name="OMM"); HMU = k.sbuf([128, NP, 3], name="HMU")
    OML = k.sbuf([128, 4], name="OML"); HML = k.sbuf([128, 4], name="HML"); OMKA = k.sbuf([128, NP, 1], name="OMKA")
    I(k, "dve", "tensor_scalar", [P], [OMM], out=OMM[:], in0=P[:, :, 9:12], scalar1=-1.0, scalar2=1.0, op0=ALU.mult, op1=ALU.add)
    I(k, "dve", "tensor_scalar", [P], [HMU], out=HMU[:], in0=P[:, :, 9:12], scalar1=0.5, scalar2=None, op0=ALU.mult)
    I(k, "dve", "tensor_scalar", [MUL], [OML], out=OML[:], in0=MUL[:], scalar1=-1.0, scalar2=1.0, op0=ALU.mult, op1=ALU.add)
    I(k, "dve", "tensor_scalar", [MUL], [HML], out=HML[:], in0=MUL[:], scalar1=0.5, scalar2=None, op0=ALU.mult)
    I(k, "dve", "tensor_scalar", [P], [OMKA], out=OMKA[:], in0=P[:, :, 5:6], scalar1=-1.0, scalar2=1.0, op0=ALU.mult, op1=ALU.add)
    ones = k.sbuf([128, SEG], name="ones")
    I(k, "dve", "memset", [], [ones], ones[:], 1.0)

    def sb(nm, shape=None):
        return k.sbuf(shape or [128, SEG], name=nm)
    ZH = sb("ZH", [128, SEG + 2]); TMPS = sb("TMPS"); RVT = sb("RVT", [128, SEG + 2])
    Rr = sb("Rr"); Kk = sb("Kk"); Vv = sb("Vv"); LR = sb("LR"); LW = sb("LW"); Aa = sb("Aa"); KKn = sb("KKn"); KS = sb("KS")
    CL = sb("CL"); EP = sb("EP"); EN = sb("EN"); T1 = sb("T1"); T2 = sb("T2")
    AR = k.sbuf([128, NCH, 2, C], name="AR")
    BDA = k.sbuf([128, NCH, 128], name="BDA"); BDB = k.sbuf([128, NCH, 128], name="BDB")
    BDK = k.sbuf([128, NCH, 128], name="BDK"); VBD = k.sbuf([128, NCH, 128], name="VBD")
    LC = k.sbuf([128, NCH], name="LC"); BASE = k.sbuf([128, NCH], name="BASE")
    for t_ in (BDA, BDB, BDK, VBD):
        I(k, "pool", "memset", [], [t_], t_[:], 0.0)
    YS = k.sbuf([128, T], name="YS"); BN = k.sbuf([128, T], name="BN")
    PN = [k.sbuf([128, 256], name=f"PN{i}") for i in range(2)]
    TT = k.sbuf([128, 256], name="TT")
    BDM = [k.sbuf([128, 128], name=f"BDM{i}") for i in range(2)]
    MRBK = [k.sbuf([128, 2, C], name=f"MRBK{i}") for i in range(2)]
    VMB = [k.sbuf([128, 128], name=f"VMB{i}") for i in range(2)]
    BKT = [k.sbuf([128, 256], name=f"BKT{i}") for i in range(2)]
    XS = k.sbuf([128, 128], name="XS"); US = [k.sbuf([128, 128], name=f"US{i}") for i in range(2)]
    SBD = [k.sbuf([128, 128], name=f"SBD{i}") for i in range(2)]
    for t_ in PN + BDM:
        I(k, "pool", "memset", [], [t_], t_[:], 0.0)
    banks = [k.psum([128, 512], name=f"rb{i}") for i in range(8)]

    def sub(b, a, c):
        return Tile(banks[b].ap[:, a:c], f"b{b}_{a}", bank=banks[b])
    PS1 = [sub(0, 0, 128), sub(0, 256, 384)]; PS2 = [sub(0, 128, 256), sub(0, 384, 512)]
    PSq = sub(1, 0, 256); PSt = sub(2, 0, 256)
    PSn = sub(3, 0, 128); PSv = sub(3, 128, 256); PSbk = sub(3, 256, 512)
    PSx = sub(4, 0, 128); PSu = sub(4, 128, 256); PSs = sub(4, 256, 384)
    PSy = [banks[5], banks[6]]
    PSb = banks[7]
    segs = seq_segments(Tc, Tl, SEG)

    def tshift(dst, src_ap_fn, seg, omm, hmu, rows=128, rev=False):
        s0, s1, q0, q1 = seg
        n = s1 - s0
        lo, hi = max(s0 - 1, q0), min(s1 + 1, q1)
        I(k, "pool", "memset", [], [ZH], ZH[:, :n + 2], 0.0)
        if rev:
            w_ = hi - lo
            k.dma("sp", RVT[:rows, 0:w_], src_ap_fn(q0 + q1 - hi, q0 + q1 - lo), writes=[RVT])
            I(k, "pool", "tensor_copy", [RVT], [ZH], out=ZH[:rows, lo - (s0 - 1):hi - (s0 - 1)], in_=RVT[:rows, 0:w_][:, ::-1])
        else:
            k.dma("sp", ZH[:rows, lo - (s0 - 1):hi - (s0 - 1)], src_ap_fn(lo, hi), writes=[ZH])
        I(k, "dve", "tensor_tensor", [ZH], [TMPS], out=TMPS[:rows, :n], in0=ZH[:rows, 0:n], in1=ZH[:rows, 2:n + 2], op=ALU.add)
        I(k, "dve", "tensor_scalar", [TMPS, HMU, HML], [TMPS], out=TMPS[:rows, :n], in0=TMPS[:rows, :n], scalar1=hmu[:rows], scalar2=None, op0=ALU.mult)
        I(k, "dve", "scalar_tensor_tensor", [ZH, OMM, OML, TMPS], [dst], out=dst[:rows, :n], in0=ZH[:rows, 1:n + 1], scalar=omm[:rows],
          in1=TMPS[:rows, :n], op0=ALU.mult, op1=ALU.add)

    ci = 0
    yi = 0
    for p in range(NP):
        for d in range(2):
            S = SBD[0]
            I(k, "dve", "memset", [], [SBD[0]], SBD[0][:], 0.0)
            si = 0
            for seg in segs:
                s0, s1, q0, q1 = seg
                n = s1 - s0
                nch = n // C
                f0 = q0 + (q1 - s1); f1 = q0 + (q1 - s0)
                rv_ = (d == 1)
                tshift(Rr, lambda lo, hi: zr[d][p, 0, :, lo:hi], seg, OMM[:, p, 0:1], HMU[:, p, 0:1], rev=rv_)
                tshift(Kk, lambda lo, hi: zr[d][p, 1, :, lo:hi], seg, OMM[:, p, 1:2], HMU[:, p, 1:2], rev=rv_)
                tshift(Vv, lambda lo, hi: zr[d][p, 2, :, lo:hi], seg, OMM[:, p, 2:3], HMU[:, p, 2:3], rev=rv_)
                tshift(LR, lambda lo, hi: zl[d][:, lo:hi], seg, OML[:, d:d + 1], HML[:, d:d + 1], rev=rv_)
                I(k, "act", "activation", [LR], [LR], out=LR[0:64, :n], in_=LR[0:64, :n], func=AF.Tanh)
                for c0 in range(0, n, 512):
                    cn = min(512, n - c0)
                    I(k, "pe", "matmul", [WUP, LR], [PSb], PSb[:, :cn], lhsT=WUP[0:64, d, p, :], rhs=LR[0:64, c0:c0 + cn], start=True, stop=True)
                    I(k, "act", "activation", [PSb, P], [LW], out=LW[:, c0:c0 + cn], in_=PSb[:, :cn], func=AF.Sigmoid, bias=P[:, p, d:d + 1], scale=1.0)
                    I(k, "pe", "matmul", [WUP, LR], [PSb], PSb[:, :cn], lhsT=WUP[64:128, d, p, :], rhs=LR[64:128, c0:c0 + cn], start=True, stop=True)
                    I(k, "act", "activation", [PSb, P], [Aa], out=Aa[:, c0:c0 + cn], in_=PSb[:, :cn], func=AF.Sigmoid, bias=P[:, p, 2 + d:3 + d], scale=1.0)
                I(k, "dve", "tensor_scalar", [LW], [LW], out=LW[:, :n], in0=LW[:, :n], scalar1=-float(np.exp(-0.5)), scalar2=None, op0=ALU.mult)
                I(k, "dve", "tensor_scalar", [Kk, P], [KKn], out=KKn[:, :n], in0=Kk[:, :n], scalar1=P[:, p, 4:5], scalar2=None, op0=ALU.mult)
                I(k, "act", "activation", [KKn], [T1], out=T1[:, :n], in_=KKn[:, :n], func=AF.Square)
                for c0 in range(0, n, 512):
                    cn = min(512, n - c0)
                    I(k, "pe", "matmul", [BLK, T1], [PSb], PSb[:, :cn], lhsT=BLK[:], rhs=T1[:, c0:c0 + cn], start=True, stop=True)
                    I(k, "dve", "tensor_scalar", [PSb], [T2], out=T2[:, c0:c0 + cn], in0=PSb[:, :cn], scalar1=1e-12, scalar2=None, op0=ALU.add)
                I(k, "act", "activation", [T2], [T2], out=T2[:, :n], in_=T2[:, :n], func=AF.Sqrt)
                I(k, "dve", "reciprocal", [T2], [T2], out=T2[:, :n], in_=T2[:, :n])
                I(k, "dve", "tensor_tensor", [KKn, T2], [KKn], out=KKn[:, :n], in0=KKn[:, :n], in1=T2[:, :n], op=ALU.mult)
                I(k, "dve", "tensor_scalar", [Aa, P, OMKA], [KS], out=KS[:, :n], in0=Aa[:, :n], scalar1=P[:, p, 5:6], scalar2=OMKA[:, p, 0:1],
                  op0=ALU.mult, op1=ALU.add)
                I(k, "dve", "tensor_tensor", [KS, Kk], [KS], out=KS[:, :n], in0=KS[:, :n], in1=Kk[:, :n], op=ALU.mult)
                I(k, "dve", "scalar_tensor_tensor", [Rr, P, KS], [T1], out=T1[:, :n], in0=Rr[:, :n], scalar=P[:, p, 6:7], in1=KS[:, :n],
                  op0=ALU.mult, op1=ALU.mult)
                for c0 in range(0, n, 512):
                    cn = min(512, n - c0)
                    I(k, "pe", "matmul", [BLK, T1], [PSb], PSb[:, :cn], lhsT=BLK[:], rhs=T1[:, c0:c0 + cn], start=True, stop=True)
                    I(k, "dve", "tensor_tensor", [PSb, Vv], [T2], out=T2[:, c0:c0 + cn], in0=PSb[:, :cn], in1=Vv[:, c0:c0 + cn], op=ALU.mult)
                if d == 0:
                    I(k, "dve", "tensor_copy", [T2], [BN], out=BN[:, s0:s1], in_=T2[:, :n])
                else:
                    I(k, "dve", "tensor_tensor", [T2, BN], [BN], out=BN[:, f0:f1], in0=BN[:, f0:f1], in1=T2[:, :n][:, ::-1], op=ALU.add)
                I(k, "dve", "tensor_tensor_scan", [ones, LW], [CL], out=CL[:, :n], data0=ones[:, :n], data1=LW[:, :n], initial=0.0,
                  op0=ALU.mult, op1=ALU.add)
                C3 = CL[:, :n].rearrange("p (c t) -> p c t", t=C)
                I(k, "dve", "memset", [], [BASE], BASE[:, 0:1], 0.0)
                if nch > 1:
                    I(k, "dve", "tensor_copy", [CL], [BASE], out=BASE[:, 1:nch], in_=C3[:, 0:nch - 1, C - 1])
                I(k, "dve", "tensor_tensor", [CL, BASE], [CL], out=C3, in0=C3, in1=BASE[:, :nch].unsqueeze(2).to_broadcast([128, nch, C]), op=ALU.subtract)
                I(k, "act", "activation", [CL], [EP], out=EP[:, :n], in_=CL[:, :n], func=AF.Exp)
                I(k, "act", "activation", [CL], [EN], out=EN[:, :n], in_=CL[:, :n], func=AF.Exp, scale=-1.0)
                I(k, "dve", "tensor_copy", [EP], [LC], out=LC[:, :nch], in_=EP[:, :n].rearrange("p (c t) -> p c t", t=C)[:, :, C - 1])
                AR4 = AR[:, :nch]
                v3 = lambda tl: tl[:, :n].rearrange("p (c t) -> p c t", t=C)
                I(k, "dve", "tensor_tensor", [Rr, EP], [AR], out=AR4[:, :, 1, :], in0=v3(Rr), in1=v3(EP), op=ALU.mult)
                I(k, "dve", "tensor_tensor", [CL, LW], [T1], out=T1[:, :n], in0=CL[:, :n], in1=LW[:, :n], op=ALU.subtract)
                I(k, "act", "activation", [T1], [T1], out=T1[:, :n], in_=T1[:, :n], func=AF.Exp)
                I(k, "dve", "scalar_tensor_tensor", [KKn, T1], [AR], out=AR4[:, :, 0, :], in0=v3(KKn), scalar=-1.0, in1=v3(T1), op0=ALU.mult, op1=ALU.mult)
                I(k, "dve", "tensor_tensor", [KKn, Aa], [T1], out=T1[:, :n], in0=KKn[:, :n], in1=Aa[:, :n], op=ALU.mult)
                I(k, "dve", "tensor_tensor", [T1, EN], [T1], out=T1[:, :n], in0=T1[:, :n], in1=EN[:, :n], op=ALU.mult)
                I(k, "dve", "tensor_tensor", [KS, EN], [T2], out=T2[:, :n], in0=KS[:, :n], in1=EN[:, :n], op=ALU.mult)
                for (bd, src, isar) in ((BDA, AR, True), (BDB, T1, False), (BDK, T2, False), (VBD, Vv, False)):
                    for hh in range(2):
                        pr = slice(hh * 64, hh * 64 + 64)
                        s_ap = AR4[pr, :, 0, :] if isar else v3(src)[pr]
                        I(k, "act" if hh else "dve", "activation" if hh else "tensor_copy", [src], [bd],
                          **({"out": bd[pr, :nch, hh * 64:hh * 64 + 64], "in_": s_ap, "func": AF.Copy} if hh else
                             {"out": bd[pr, :nch, hh * 64:hh * 64 + 64], "in_": s_ap}))
                for c in range(nch if dbg >= 2 else 0):
                    a = ci % 2
                    ci += 1
                    ARc = AR[:, c].rearrange("p a t -> p (a t)")
                    I(k, "pe", "matmul", [BDB, AR], [PS1[a]], PS1[a][:, :], lhsT=BDB[:, c, :], rhs=ARc, start=True, stop=True)
                    if dbg == 2 and SUBDBG >= 1:
                        I(k, "pe", "matmul", [BDK, AR], [PS2[a]], PS2[a][:, :], lhsT=BDK[:, c, :], rhs=ARc, start=True, stop=True)
                    if dbg == 2 and SUBDBG < 2:
                        continue
                    if dbg > 2:
                        I(k, "pe", "matmul", [BDK, AR], [PS2[a]], PS2[a][:, :], lhsT=BDK[:, c, :], rhs=ARc, start=True, stop=True)
                    pn = PN[0]
                    if dbg == 2 and ci > int(os.environ.get('LIMC', '99999')):
                        continue
                    for hh in (range(2) if dbg > 2 else {'1': [0], '2': [0, 1], '3': [1], '4': [1, 0]}[os.environ.get('LIMH', '2')]):
                        pr = slice(hh * 64, hh * 64 + 64)
                        if os.environ.get('NOMS'):
                            I(k, "dve", "tensor_copy", [PS1[a], PS2[a]], [pn], out=pn[pr, hh * 64:hh * 64 + 64], in_=PS1[a][pr, 0:64])
                        else:
                            I(k, "dve", "tensor_tensor", [PS1[a], PS2[a], MS], [pn], out=pn[pr, hh * 64:hh * 64 + 64], in0=PS1[a][pr, 0:64], in1=MS[pr, 0:64], op=ALU.mult)
                        if dbg == 2 and SUBDBG < 3:
                            continue
                        I(k, "dve", "tensor_tensor", [PS2[a], MS], [BDM[a]], out=BDM[a][pr, hh * 64:hh * 64 + 64], in0=PS2[a][pr, 0:64], in1=MS[pr, 0:64], op=ALU.mult)
                    if dbg == 2 and SUBDBG < 4:
                        continue
                    I(k, "dve", "tensor_tensor", [PS1[a], MS], [MRBK[a]], out=MRBK[a][:, 0, :], in0=PS1[a][:, 64:128], in1=MS[:, 64:128], op=ALU.mult)
                    I(k, "dve", "tensor_tensor", [PS2[a], MS], [MRBK[a]], out=MRBK[a][:, 1, :], in0=PS2[a][:, 64:128], in1=MS[:, 64:128], op=ALU.mult)
                    if dbg < 3:
                        continue
                    I(k, "pe", "transpose", [pn, IDN2], [PSn], PSn[:, :], pn[:, 0:128], IDN2[:, 0:128])
                    I(k, "act", "activation", [PSn], [pn], out=pn[:, 128:256], in_=PSn[:, :], func=AF.Copy)
                    I(k, "dve", "tensor_tensor", [pn, IDN2], [TT], out=TT[:], in0=pn[:], in1=IDN2[:], op=ALU.add)
                    cur = 0
                    for lvl in range(1, 6):
                        last = (lvl == 5)
                        pc = PN[cur]; pnx = PN[1 - cur]
                        I(k, "pe", "matmul", [pc], [PSq], PSq[:, 0:128], lhsT=pc[:, 128:256], rhs=pc[:, 0:128], start=True, stop=True)
                        if not last:
                            I(k, "pe", "matmul", [pc], [PSq], PSq[:, 128:256], lhsT=pc[:, 0:128], rhs=pc[:, 128:256], start=True, stop=True)
                        w_ = 128 if last else 256
                        I(k, "act", "activation", [PSq], [pnx], out=pnx[:, 0:w_], in_=PSq[:, 0:w_], func=AF.Copy)
                        I(k, "pe", "matmul", [TT, pnx], [PSt], PSt[:, 0:128], lhsT=TT[:, 128:256], rhs=pnx[:, 0:128], start=True, stop=True)
                        if not last:
                            I(k, "pe", "matmul", [TT, pnx], [PSt], PSt[:, 128:256], lhsT=pnx[:, 0:128], rhs=TT[:, 128:256], start=True, stop=True)
                        I(k, "dve", "tensor_tensor", [PSt, TT], [TT], out=TT[:, 0:w_], in0=PSt[:, 0:w_], in1=TT[:, 0:w_], op=ALU.add)
                        cur = 1 - cur
                    if dbg < 4:
                        continue
                    I(k, "pe", "transpose", [VBD, IDN2], [PSv], PSv[:, :], VBD[:, c, :], IDN2[:, 0:128])
                    I(k, "act", "activation", [PSv], [VMB[a]], out=VMB[a][:], in_=PSv[:, :], func=AF.Copy)
                    I(k, "pe", "transpose", [BDB, IDN2], [PSbk], PSbk[:, 0:128], BDB[:, c, :], IDN2[:, 0:128])
                    I(k, "pe", "transpose", [BDK, IDN2], [PSbk], PSbk[:, 128:256], BDK[:, c, :], IDN2[:, 0:128])
                    I(k, "act", "activation", [PSbk], [BKT[a]], out=BKT[a][:], in_=PSbk[:, :], func=AF.Copy)
                    if dbg < 5:
                        continue
                    Sc = SBD[si % 2]; Sn = SBD[(si + 1) % 2]; si += 1
                    I(k, "pe", "matmul", [BDA, Sc], [PSx], PSx[:, :], lhsT=BDA[:, c, :], rhs=Sc[:], start=True, stop=False)
                    I(k, "pe", "matmul", [BDM[a], VMB[a]], [PSx], PSx[:, :], lhsT=BDM[a][:], rhs=VMB[a][:], start=False, stop=True)
                    I(k, "act", "activation", [PSx], [XS], out=XS[:], in_=PSx[:, :], func=AF.Copy)
                    I(k, "pe", "matmul", [TT, XS], [PSu], PSu[:, :], lhsT=TT[:, 0:128], rhs=XS[:], start=True, stop=True)
                    I(k, "act", "activation", [PSu], [US[a]], out=US[a][:], in_=PSu[:, :], func=AF.Copy)
                    if c % 8 == 0:
                        py = PSy[yi % 2]; yi += 1
                    oc = (c % 8) * C
                    I(k, "pe", "matmul", [Sc, AR], [py], py[:, oc:oc + C], lhsT=Sc[:], rhs=AR[:, c, 1, :], start=True, stop=False)
                    I(k, "pe", "matmul", [US[a], MRBK[a]], [py], py[:, oc:oc + C], lhsT=US[a][:], rhs=MRBK[a][:, 0, :], start=False, stop=False)
                    I(k, "pe", "matmul", [VMB[a], MRBK[a]], [py], py[:, oc:oc + C], lhsT=VMB[a][:], rhs=MRBK[a][:, 1, :], start=False, stop=True)
                    I(k, "pe", "matmul", [BKT[a], US[a]], [PSs], PSs[:, :], lhsT=BKT[a][:, 0:128], rhs=US[a][:], start=True, stop=False)
                    I(k, "pe", "matmul", [BKT[a], VMB[a]], [PSs], PSs[:, :], lhsT=BKT[a][:, 128:256], rhs=VMB[a][:], start=False, stop=True)
                    I(k, "dve", "tensor_tensor", [PSs, Sc], [Sn], out=Sn[:], in0=PSs[:, :], in1=Sc[:], op=ALU.add)
                    I(k, "dve", "tensor_scalar", [Sn, LC], [Sn], out=Sn[:], in0=Sn[:], scalar1=LC[:, c:c + 1], scalar2=None, op0=ALU.mult)
                    if c % 8 == 7 or c == nch - 1:
                        c0 = (c // 8) * 8 * C
                        w = (c % 8 + 1) * C
                        if d == 0:
                            I(k, "act", "activation", [py], [YS], out=YS[:, s0 + c0:s0 + c0 + w], in_=py[:, 0:w], func=AF.Copy)
                        else:
                            I(k, "dve", "tensor_tensor", [py, YS], [YS], out=YS[:, f1 - c0 - w:f1 - c0], in0=YS[:, f1 - c0 - w:f1 - c0],
                              in1=py[:, 0:w][:, ::-1], op=ALU.add)
                if si % 2 == 1:
                    pass
        G0 = Rr; G1 = Kk; GG = Vv; YC = KS; SQ = T1; RS = T2; MN = CL
        for seg in (segs if dbg >= 6 else []):
            s0, s1, q0, q1 = seg
            n = s1 - s0
            tshift(G0, lambda lo, hi: zgd[0, :, lo:hi], seg, OML[:, 2:3], HML[:, 2:3])
            tshift(G1, lambda lo, hi: zgd[1, 0:32, lo:hi], seg, OML[:, 3:4], HML[:, 3:4], rows=32)
            I(k, "act", "activation", [G0], [G0], out=G0[:, :n], in_=G0[:, :n], func=AF.Sigmoid)
            I(k, "act", "activation", [G1], [G1], out=G1[0:32, :n], in_=G1[0:32, :n], func=AF.Sigmoid)
            for c0 in range(0, n, 512):
                cn = min(512, n - c0)
                I(k, "pe", "matmul", [GUP, G0], [PSb], PSb[:, :cn], lhsT=GUP[:, 0, p, :], rhs=G0[:, c0:c0 + cn], start=True, stop=False)
                I(k, "pe", "matmul", [GUP, G1], [PSb], PSb[:, :cn], lhsT=GUP[0:32, 1, p, :], rhs=G1[0:32, c0:c0 + cn], start=False, stop=True)
                I(k, "act", "activation", [PSb], [GG], out=GG[:, c0:c0 + cn], in_=PSb[:, :cn], func=AF.Copy)
                I(k, "pe", "matmul", [BLK, YS], [PSb], PSb[:, :cn], lhsT=BLK[:], rhs=YS[:, s0 + c0:s0 + c0 + cn], start=True, stop=True)
                I(k, "dve", "scalar_tensor_tensor", [PSb, YS], [YC], out=YC[:, c0:c0 + cn], in0=PSb[:, :cn], scalar=-1.0 / 64, in1=YS[:, s0 + c0:s0 + c0 + cn],
                  op0=ALU.mult, op1=ALU.add)
                I(k, "act", "activation", [YC], [SQ], out=SQ[:, c0:c0 + cn], in_=YC[:, c0:c0 + cn], func=AF.Square)
                I(k, "pe", "matmul", [BLK, SQ], [PSb], PSb[:, :cn], lhsT=BLK[:], rhs=SQ[:, c0:c0 + cn], start=True, stop=True)
                I(k, "dve", "tensor_scalar", [PSb], [RS], out=RS[:, c0:c0 + cn], in0=PSb[:, :cn], scalar1=1.0 / 64, scalar2=64e-5, op0=ALU.mult, op1=ALU.add)
            I(k, "act", "activation", [RS], [RS], out=RS[:, :n], in_=RS[:, :n], func=AF.Sqrt)
            I(k, "dve", "reciprocal", [RS], [RS], out=RS[:, :n], in_=RS[:, :n])
            I(k, "dve", "tensor_tensor", [YC, RS], [YC], out=YC[:, :n], in0=YC[:, :n], in1=RS[:, :n], op=ALU.mult)
            I(k, "dve", "tensor_scalar", [YC, P], [YC], out=YC[:, :n], in0=YC[:, :n], scalar1=P[:, p, 7:8], scalar2=P[:, p, 8:9], op0=ALU.mult, op1=ALU.add)
            I(k, "dve", "tensor_tensor", [YC, BN], [YC], out=YC[:, :n], in0=YC[:, :n], in1=BN[:, s0:s1], op=ALU.add)
            I(k, "dve", "tensor_tensor", [YC, GG], [YC], out=YC[:, :n], in0=YC[:, :n], in1=GG[:, :n], op=ALU.mult)
            k.dma("pool", yo[p, :, s0:s1], YC[:, :n], reads=[YC], is_output=True)
    k.finish()
    return k


B_, S_, D_, L_, TC_ = 2, 8192, 4096, 4, 256
GW_ = 1024
T_ = TC_ + S_
OFF_HG, OFF_RW, OFF_LRU, OFF_POOL = 0, 5120, 8608, 10656
_PROG = {}
N_LAUNCH = [0]


import time as _time
_VERB = bool(os.environ.get("KVERB"))


def _prog(name, fn):
    if name not in _PROG:
        t = _time.time()
        _PROG[name] = fn()
        if _VERB:
            print(f"[build {name}] {_time.time() - t:.1f}s instr={_PROG[name].n_instr}", flush=True)
    return _PROG[name]


def _run(k, in_maps):
    N_LAUNCH[0] += 1
    t = _time.time()
    res = run_bass_kernel_spmd(k.nc, in_maps, core_ids=list(range(len(in_maps))), **({"trace": True} if os.environ.get("KTRACE") else {}))
    if _VERB:
        nb = sum(v.nbytes for m in in_maps for v in m.values())
        print(f"[run] {_time.time() - t:.1f}s in_bytes={nb / 1e6:.0f}MB exec_ns={getattr(res, 'exec_time_ns', None)}", flush=True)
    return res.results


def _c(a):
    return np.ascontiguousarray(a, dtype=np.float32)


def rw_consts():
    idn2 = np.concatenate([np.eye(128), np.eye(128)], 1).astype(np.float32)
    msk = np.zeros((128, 128), np.float32)
    for p in range(128):
        i = p % 64
        msk[p, 0:64] = (np.arange(64) > i)
        msk[p, 64:128] = (np.arange(64) >= i)
    blk = np.zeros((128, 128), np.float32)
    blk[:64, :64] = 1
    blk[64:, 64:] = 1
    return {"idn2": idn2, "msk": msk, "blk": blk}


def rw_inputs(z, zf, j, prm):
    mu, w0, w_up, a0, a_up, g_up, k_k, k_a, r_k, ln_w, ln_b = prm
    T = z.shape[0]
    ins = {}
    zr = np.zeros((2, 3, 128, T), np.float32)
    for p in range(2):
        for i in range(3):
            c0 = i * 1024 + j * 256 + p * 128
            zr[p, i] = z[:, c0:c0 + 128].T
    ins["zr0"] = zr
    for d in range(2):
        ins[f"zl{d}"] = _c(np.concatenate([z[:, 3072 + 64 * d:3136 + 64 * d], z[:, 3200 + 64 * d:3264 + 64 * d]], 1).T)
    zgd = np.zeros((2, 128, T), np.float32)
    zgd[0] = z[:, 3328:3456].T
    zgd[1, :32] = z[:, 3456:3488].T
    ins["zgd"] = zgd
    P = np.zeros((128, 2, 12), np.float32)
    for p in range(2):
        ch = slice(j * 256 + p * 128, j * 256 + p * 128 + 128)
        cols = [w0[0][ch], w0[1][ch], a0[0][ch], a0[1][ch], k_k[ch], k_a[ch], r_k[ch], ln_w[ch], ln_b[ch],
                mu[0:1024][ch], mu[1024:2048][ch], mu[2048:3072][ch]]
        P[:, p, :] = np.stack(cols, 1)
    ins["prm"] = P
    mul = np.zeros((128, 4), np.float32)
    for d in range(2):
        mul[:, d] = np.concatenate([mu[3072 + 64 * d:3136 + 64 * d], mu[3200 + 64 * d:3264 + 64 * d]])
    mul[:, 2] = mu[3328:3456]
    mul[:32, 3] = mu[3456:3488]
    ins["mul"] = mul
    wup = np.zeros((128, 2, 2, 128), np.float32)
    gup = np.zeros((128, 2, 2, 128), np.float32)
    for p in range(2):
        ch = slice(j * 256 + p * 128, j * 256 + p * 128 + 128)
        for d in range(2):
            wup[:64, d, p] = w_up[d][:, ch]
            wup[64:, d, p] = a_up[d][:, ch]
        gup[:, 0, p] = g_up[0:128, ch]
        gup[:32, 1, p] = g_up[128:160, ch]
    ins["wup"] = wup
    ins["gup"] = gup
    ins.update(rw_consts())
    return ins


def hg_inputs(z, zf, j, layer, hg_lb, gnw):
    gw = GW_
    L = hg_lb.shape[1]

    def part(zz, idx):
        return zz[:, idx * gw + j * 256: idx * gw + (j + 1) * 256].T.reshape(2, 128, -1)
    zd0 = np.stack([part(z, 0), part(z, 1), part(z, 3)], 1)
    zfb = part(z, 2)
    zg = part(z, 4)
    lbp = _c(hg_lb[:, :, j * 256:(j + 1) * 256].reshape(2, L, 2, 128).transpose(3, 2, 0, 1))
    lmask = np.zeros((128, L), np.float32)
    lmask[:, 1:layer + 1] = 1
    msk = np.triu(np.ones((HG_C, HG_C), np.float32))
    return {"zd0": _c(zd0), "zfb": _c(zfb), "zg": _c(zg), "lbp": lbp, "lmask": lmask,
            "gn": _c(gnw.reshape(128, 1)), "msk": msk, "idn": np.eye(128, dtype=np.float32)}


def lru_inputs(z, j, conv_w, conv_b, wa, ba, wx, bx, lam):
    gw = GW_
    ch = slice(j * 256, (j + 1) * 256)
    zx = _c(z[:, :gw][:, ch].T.reshape(2, 128, -1))
    zy = _c(z[:, gw:][:, ch].T.reshape(2, 128, -1))
    cols = np.stack([conv_w[0], conv_w[1], conv_w[2], conv_w[3], conv_b, ba[0], ba[1], bx[0], bx[1], lam[0], lam[1]], -1)
    prm = _c(cols[ch].reshape(2, 128, 11).transpose(1, 0, 2))
    WA = _c(wa[:, 2 * j:2 * j + 2].transpose(2, 1, 0, 3))
    WX = _c(wx[:, 2 * j:2 * j + 2].transpose(2, 1, 0, 3))
    return {"zx": zx, "zy": zy, "prm": prm, "wa": WA, "wx": WX}


def _pad_lines(a):
    ch, nl, lw = a.shape
    o = np.zeros((ch, nl, lw + 16), np.float32)
    o[:, :, 8:8 + lw] = a
    return o.reshape(8, 128, nl, lw + 16)


def pool_inputs(zl, zc, j, layer, lin_w, scale):
    S_ = zl.shape[0]
    TC_ = zc.shape[0]
    rows = S_ // 64
    g3 = zl.reshape(rows, 64, GW_)
    if layer % 2 == 0:
        nl = rows // 4
        lines = g3[j * nl:(j + 1) * nl].transpose(2, 0, 1)
        n_line = 64
    else:
        nl = 64 // 4
        lines = g3[:, j * nl:(j + 1) * nl].transpose(2, 1, 0)
        n_line = rows
    cpad = np.zeros((TC_ + 16, GW_), np.float32)
    cpad[8:8 + TC_] = zc
    seg = TC_ // 4
    zp1 = _c(cpad[j * seg:j * seg + seg + 16].T.reshape(8, 128, 1, seg + 16))
    inv0 = np.tile(pool_invcount(n_line, np.arange(n_line))[None], (128, 1, 1))
    inv1 = np.tile(pool_invcount(TC_, np.arange(j * seg, (j + 1) * seg))[None], (128, 1, 1))
    LIN = _c(lin_w.reshape(4, 2, 128, 256).transpose(2, 0, 1, 3))
    SCL = _c(scale.reshape(8, 128).T)
    return {"zp0": _c(_pad_lines(lines)), "zp1": zp1, "inv0": _c(inv0), "inv1": _c(inv1), "lin": LIN, "scl": SCL}


def kernel(x, c, ctx, c_ctx, w_ada, b_ada, norm_g, w_in, w_out, hg_lb, hg_gnorm, rw_mu, rw_w0, rw_w_up, rw_a0, rw_a_up,
           rw_g_up, rw_kk, rw_ka, rw_rk, rw_ln_w, rw_ln_b, lru_conv_w, lru_conv_b, lru_wa, lru_ba, lru_wx, lru_bx,
           lru_lam, pool_w, pool_scale, moe_wc, moe_bc, moe_wf, moe_bf, moe_w_gu, moe_w_down, final_g):
    A = lambda a: np.asarray(a, dtype=np.float32)
    x, c, ctx, c_ctx = A(x), A(c), A(ctx), A(c_ctx)
    NC = 8
    B_, S_, D_ = x.shape
    TC_ = ctx.shape[1]
    L_ = w_in.shape[0]
    T_ = TC_ + S_
    cfg = Cfg(ntiles_lat=S_ // 4 // 512, nctx=TC_ // 4)
    NCOL = 6 * D_ // NC
    k0 = _prog(f"P0_{L_}", lambda: build_P0(D_, NCOL // 128, L_))
    crow = np.stack([c[0], c[1], c_ctx])
    cT = _c(crow.T.reshape(D_ // 128, 128, 3).transpose(1, 0, 2))
    maps = []
    for i in range(NC):
        wl = np.stack([wlayout(A(w_ada[l])[:, i * NCOL:(i + 1) * NCOL]) for l in range(L_)])
        bl = _c(A(b_ada)[:, i * NCOL:(i + 1) * NCOL].reshape(L_, NCOL // 128, 128).transpose(2, 0, 1))
        maps.append({"w": wl, "cT": cT, "b": bl})
    res = _run(k0, maps)
    mod = np.zeros((L_, 3, 6 * D_), np.float32)
    for i in range(NC):
        o = res[i]["out"]
        mod[:, :, i * NCOL:(i + 1) * NCOL] = o.transpose(1, 3, 2, 0).reshape(L_, 3, NCOL)
    del maps, res
    mod = mod.reshape(L_, 3, 6, D_)

    xl = x.copy()
    xc = ctx.copy()
    nlat = S_ // 4
    nctx = TC_ // 4

    def tok_slice(arr_l, arr_c, b, j):
        return np.concatenate([arr_l[b, j * nlat:(j + 1) * nlat], arr_c[b, j * nctx:(j + 1) * nctx]], 0)

    sel = np.zeros((16, 16, 128), np.float32)
    for e in range(16):
        sel[e, e, :] = 1
    idn = np.eye(128, dtype=np.float32)

    for l in range(L_):
        kA = _prog(f"A{S_}", lambda: build_A(cfg))
        wl = wlayout(A(w_in[l]))
        maps = []
        for b in range(B_):
            for j in range(4):
                prm = np.stack([np.stack([mod[l, b, 0], mod[l, b, 1], A(norm_g)[l, 0]]),
                                np.stack([mod[l, 2, 0], mod[l, 2, 1], A(norm_g)[l, 0]])])
                maps.append({"xT": fm(tok_slice(xl, xc, b, j)), "w": wl, "prm": vec_fm(prm)})
        res = _run(kA, maps)
        W = cfg.NCC_IN * 128
        zl = np.zeros((B_, S_, W), np.float32)
        zc = np.zeros((B_, TC_, W), np.float32)
        for b in range(B_):
            for j in range(4):
                zt = res[b * 4 + j]["zT"].reshape(W, cfg.NTOK).T
                zl[b, j * nlat:(j + 1) * nlat] = zt[:nlat]
                zc[b, j * nctx:(j + 1) * nctx] = zt[nlat:]
        del maps, res, wl
        y_l = np.zeros((B_, S_, 4 * GW_), np.float32)
        y_c = np.zeros((B_, TC_, 4 * GW_), np.float32)
        zcat = [np.concatenate([zc[b], zl[b]], 0) for b in range(B_)]
        zflip = [None] * B_

        def put(mix, b, j, yT):
            c0 = mix * GW_ + j * 256
            y_c[b][:, c0:c0 + 256] = yT[:, :TC_].T
            y_l[b][:, c0:c0 + 256] = yT[:, TC_:].T

        kH = _prog(f"HG{S_}", lambda: build_HG(TC_, S_, 1024, L=L_))
        maps = [hg_inputs(zcat[b][:, OFF_HG:OFF_RW], None, j, l, A(hg_lb), A(hg_gnorm)[l])
                for b in range(B_) for j in range(4)]
        res = _run(kH, maps)
        for b in range(B_):
            for j in range(4):
                put(0, b, j, res[b * 4 + j]["yo"].reshape(256, T_))
        kR = _prog(f"RW{S_}", lambda: build_RW(TC_, S_, 512))
        rprm = (A(rw_mu)[l], A(rw_w0)[l], A(rw_w_up)[l], A(rw_a0)[l], A(rw_a_up)[l], A(rw_g_up)[l], A(rw_kk)[l],
                A(rw_ka)[l], A(rw_rk)[l], A(rw_ln_w)[l], A(rw_ln_b)[l])
        maps = [rw_inputs(zcat[b][:, OFF_RW:OFF_LRU], None, j, rprm) for b in range(B_) for j in range(4)]
        res = _run(kR, maps)
        for b in range(B_):
            for j in range(4):
                put(1, b, j, res[b * 4 + j]["yo"].reshape(256, T_))
        kL = _prog(f"LRU{S_}", lambda: build_LRU(TC_, S_, 2048))
        maps = [lru_inputs(zcat[b][:, OFF_LRU:OFF_POOL], j, A(lru_conv_w)[l], A(lru_conv_b)[l], A(lru_wa)[l], A(lru_ba)[l],
                           A(lru_wx)[l], A(lru_bx)[l], A(lru_lam)[l]) for b in range(B_) for j in range(4)]
        res = _run(kL, maps)
        for b in range(B_):
            for j in range(4):
                put(2, b, j, res[b * 4 + j]["yo"].reshape(256, T_))
        rows = S_ // 64
        grp = [(rows // 4, 64), (1, TC_ // 4)] if l % 2 == 0 else [(16, rows), (1, TC_ // 4)]
        kP = _prog(f"POOL{l % 2}_{S_}", lambda: build_POOL(grp))
        maps = [pool_inputs(zl[b][:, OFF_POOL:OFF_POOL + GW_], zc[b][:, OFF_POOL:OFF_POOL + GW_], j, l, A(pool_w)[l], A(pool_scale)[l])
                for b in range(B_) for j in range(4)]
        res = _run(kP, maps)
        for b in range(B_):
            yg = y_l[b].reshape(rows, 64, 4 * GW_)
            for j in range(4):
                r0 = res[b * 4 + j]["yp0"]
                if l % 2 == 0:
                    nl = rows // 4
                    yg[j * nl:(j + 1) * nl, :, 3 * GW_:] = r0.reshape(GW_, nl, 64).transpose(1, 2, 0)
                else:
                    nl = 16
                    yg[:, j * nl:(j + 1) * nl, 3 * GW_:] = r0.reshape(GW_, nl, rows).transpose(2, 1, 0)
                y_c[b][j * nctx:(j + 1) * nctx, 3 * GW_:] = res[b * 4 + j]["yp1"].reshape(GW_, nctx).T
        del maps, res, zcat, zflip, zl, zc
        final = (l == L_ - 1)
        kC = _prog(("C1" if final else "C0") + str(S_), lambda: build_C(cfg, final=final))
        wo = wlayout(A(w_out[l]))
        wgu = A(moe_w_gu[l])
        wgu_l = np.stack([wlayout(wgu[e]) for e in range(16)])
        wdn = wlayout(A(moe_w_down[l]).reshape(16 * 256, D_))
        wcf = np.concatenate([A(moe_wc[l]), A(moe_wf[l])], 1)
        wcf = _c(wcf.reshape(D_ // 128, 128, 20).transpose(1, 0, 2))
        bcf = _c(np.tile(np.concatenate([A(moe_bc[l]), A(moe_bf[l])])[None], (128, 1)))
        maps = []
        for b in range(B_):
            for j in range(4):
                g1 = A(norm_g)[l, 1]
                fg = A(final_g)
                prm = np.stack([np.stack([mod[l, b, 2], mod[l, b, 3], mod[l, b, 4], mod[l, b, 5], g1, fg]),
                                np.stack([mod[l, 2, 2], mod[l, 2, 3], mod[l, 2, 4], mod[l, 2, 5], g1, fg])])
                maps.append({"xT": fm(tok_slice(xl, xc, b, j)), "yT": fm(tok_slice(y_l, y_c, b, j)), "w_out": wo,
                             "prm": vec_fm(prm), "wcf": wcf, "bcf": bcf, "w_gu": wgu_l, "w_dn": wdn, "sel": sel, "idn": idn})
        res = _run(kC, maps)
        for b in range(B_):
            for j in range(4):
                xt = unfm(res[b * 4 + j]["xo"])
                xl[b, j * nlat:(j + 1) * nlat] = xt[:nlat]
                xc[b, j * nctx:(j + 1) * nctx] = xt[nlat:]
        del maps, res, wo, wgu_l, wdn, y_l, y_c
    return xl
```
